# Optimizing a Trainium2 kernel written in Bass

```python
import math
import jax, jax.numpy as jnp
from jax import lax
import numpy as np

D_MODEL = 1024
BATCH = 8
SEQ = 2048
DEPTH = 2

HEAD_DIM = 64
A_Q_HEADS = 8
A_KV_HEADS = 2
A_GROUP = A_Q_HEADS // A_KV_HEADS
WINDOW = 128
B_HEADS = 8
MOBA_BLOCK = 256
MOBA_TOPK = 3
Q_CHUNK = 64
N_ALIBI = A_Q_HEADS + B_HEADS
D_FF = -(-(8 * D_MODEL) // (3 * 256)) * 256
N_MOD = 6
EPS = 1e-6

A_Q_W = A_Q_HEADS * HEAD_DIM
A_KV_W = A_KV_HEADS * HEAD_DIM
B_W = B_HEADS * HEAD_DIM
IN_WIDTHS = (A_Q_W, A_KV_W, A_KV_W, B_W, B_W, B_W, D_MODEL, D_MODEL)
IN_COLS = sum(IN_WIDTHS)

kernel_name = "hybrid_swa_sink_moba_gated_block"


def rms_norm(x, g):
    xf = x.astype(jnp.float32)
    y = xf * lax.rsqrt(jnp.mean(xf * xf, axis=-1, keepdims=True) + EPS)
    return (y * g.astype(jnp.float32)).astype(x.dtype)


def alibi_slopes():
    i = np.arange(1, N_ALIBI + 1, dtype=np.float32)
    s = jnp.asarray(2.0 ** (-8.0 * i / N_ALIBI), dtype=jnp.float32)
    return s[:A_Q_HEADS], s[A_Q_HEADS:]


def sliding_window_attention(q, k, v, sinks, slopes):
    B, S = q.shape[0], q.shape[1]
    nb = S // WINDOW
    scale = HEAD_DIM ** -0.5
    qb = q.reshape(B, nb, WINDOW, A_KV_HEADS, A_GROUP, HEAD_DIM)
    kb = k.reshape(B, nb, WINDOW, A_KV_HEADS, HEAD_DIM)
    vb = v.reshape(B, nb, WINDOW, A_KV_HEADS, HEAD_DIM)
    pad = ((0, 0), (1, 0), (0, 0), (0, 0), (0, 0))
    kc = jnp.concatenate([jnp.pad(kb, pad)[:, :-1], kb], axis=2)
    vc = jnp.concatenate([jnp.pad(vb, pad)[:, :-1], vb], axis=2)
    scores = jnp.einsum('bnqhgd,bnkhd->bhgnqk', qb, kc).astype(jnp.float32) * scale
    t = jnp.arange(WINDOW)[:, None] + WINDOW
    s = jnp.arange(2 * WINDOW)[None, :]
    dist = (t - s).astype(jnp.float32)
    valid = ((t - s) >= 0) & ((t - s) < WINDOW)
    block_ok = (jnp.arange(nb)[:, None, None] > 0) | (s[None] >= WINDOW)
    mask = valid[None] & block_ok
    m_h = slopes.reshape(1, A_KV_HEADS, A_GROUP, 1, 1, 1)
    scores = jnp.where(mask, scores - m_h * dist, -jnp.inf)
    sink = sinks.astype(jnp.float32).reshape(1, A_KV_HEADS, A_GROUP, 1, 1, 1)
    m = jnp.maximum(jnp.max(scores, axis=-1, keepdims=True), sink)
    p = jnp.exp(scores - m)
    denom = jnp.sum(p, axis=-1, keepdims=True) + jnp.exp(sink - m)
    out = jnp.einsum('bhgnqk,bnkhd->bnqhgd', (p / denom).astype(v.dtype), vc)
    return out.reshape(B, S, A_Q_W)


def moba_attention(q, k, v, slopes):
    B, S = q.shape[0], q.shape[1]
    Sp = -(-S // MOBA_BLOCK) * MOBA_BLOCK
    pad = ((0, 0), (0, Sp - S), (0, 0), (0, 0))
    q, k, v = jnp.pad(q, pad), jnp.pad(k, pad), jnp.pad(v, pad)
    nb = Sp // MOBA_BLOCK
    k_top = min(MOBA_TOPK, nb)
    nc = Sp // Q_CHUNK
    scale = HEAD_DIM ** -0.5
    kb = k.transpose(0, 2, 1, 3).reshape(B, B_HEADS, nb, MOBA_BLOCK, HEAD_DIM)
    vb = v.transpose(0, 2, 1, 3).reshape(B, B_HEADS, nb, MOBA_BLOCK, HEAD_DIM)
    k_mean = jnp.mean(kb.astype(jnp.float32), axis=3)
    q_chunks = jnp.moveaxis(q.transpose(0, 2, 1, 3).reshape(B, B_HEADS, nc, Q_CHUNK, HEAD_DIM), 2, 0)
    starts = jnp.arange(nc, dtype=jnp.int32) * Q_CHUNK
    bi = jnp.arange(B)[:, None, None]
    hi = jnp.arange(B_HEADS)[None, :, None]
    m_own = slopes.reshape(1, B_HEADS, 1, 1)
    m_sel = slopes.reshape(1, B_HEADS, 1, 1, 1)
    blk_pos = jnp.arange(MOBA_BLOCK, dtype=jnp.int32)

    def chunk_fn(args):
        q_c, start = args
        t = start + jnp.arange(Q_CHUNK, dtype=jnp.int32)
        own = start // MOBA_BLOCK
        gate = jnp.einsum('bhqd,bhnd->bhqn', q_c.astype(jnp.float32), k_mean)
        gate = jnp.where(jnp.arange(nb) < own, gate, -jnp.inf)
        _, idx = lax.top_k(gate, k_top)
        sel_ok = jnp.arange(k_top) < own
        k_own = lax.dynamic_index_in_dim(kb, own, axis=2, keepdims=False)
        v_own = lax.dynamic_index_in_dim(vb, own, axis=2, keepdims=False)
        d_o = t[:, None] - (own * MOBA_BLOCK + blk_pos)[None, :]
        s_o = jnp.einsum('bhqd,bhkd->bhqk', q_c, k_own).astype(jnp.float32) * scale
        s_o = jnp.where(d_o >= 0, s_o - m_own * d_o.astype(jnp.float32), -jnp.inf)
        flat = idx.reshape(B, B_HEADS, Q_CHUNK * k_top)
        k_sel = kb[bi, hi, flat].reshape(B, B_HEADS, Q_CHUNK, k_top, MOBA_BLOCK, HEAD_DIM)
        v_sel = vb[bi, hi, flat].reshape(B, B_HEADS, Q_CHUNK, k_top, MOBA_BLOCK, HEAD_DIM)
        s_s = jnp.einsum('bhqd,bhqrkd->bhqrk', q_c, k_sel).astype(jnp.float32) * scale
        d_s = t[None, None, :, None, None] - (idx[..., None] * MOBA_BLOCK + blk_pos)
        s_s = jnp.where(sel_ok[:, None], s_s - m_sel * d_s.astype(jnp.float32), -jnp.inf)
        m = jnp.maximum(jnp.max(s_o, axis=-1), jnp.max(s_s, axis=(-2, -1)))[..., None]
        p_o = jnp.exp(s_o - m)
        p_s = jnp.exp(s_s - m[..., None])
        denom = jnp.sum(p_o, axis=-1) + jnp.sum(p_s, axis=(-2, -1))
        num = (jnp.einsum('bhqk,bhkd->bhqd', p_o.astype(v.dtype), v_own).astype(jnp.float32)
               + jnp.einsum('bhqrk,bhqrkd->bhqd', p_s.astype(v.dtype), v_sel).astype(jnp.float32))
        return (num / denom[..., None]).astype(q_c.dtype)

    out = lax.map(chunk_fn, (q_chunks, starts))
    out = jnp.moveaxis(out, 0, 2).reshape(B, B_HEADS, Sp, HEAD_DIM)
    return out.transpose(0, 2, 1, 3).reshape(B, Sp, B_W)[:, :S]


def mixer(h, w_in, sinks, w_o_a, w_o_b, w_out):
    B, S, _ = h.shape
    proj = h @ w_in
    offsets = [int(o) for o in np.cumsum(IN_WIDTHS)[:-1]]
    a_q, a_k, a_v, b_q, b_k, b_v, g_a, g_b = jnp.split(proj, offsets, axis=-1)
    slopes_a, slopes_b = alibi_slopes()
    o_a = sliding_window_attention(
        a_q.reshape(B, S, A_Q_HEADS, HEAD_DIM), a_k.reshape(B, S, A_KV_HEADS, HEAD_DIM),
        a_v.reshape(B, S, A_KV_HEADS, HEAD_DIM), sinks, slopes_a) @ w_o_a
    o_b = moba_attention(
        b_q.reshape(B, S, B_HEADS, HEAD_DIM), b_k.reshape(B, S, B_HEADS, HEAD_DIM),
        b_v.reshape(B, S, B_HEADS, HEAD_DIM), slopes_b) @ w_o_b
    merged = jax.nn.sigmoid(g_a) * o_a + jax.nn.sigmoid(g_b) * o_b
    return merged @ w_out


def swiglu(h, w_gate_up, w_down):
    gate, up = jnp.split(h @ w_gate_up, 2, axis=-1)
    return (jax.nn.silu(gate) * up) @ w_down


def setup_inputs(seed: int = 0) -> dict:
    key = jax.random.key(seed)
    ks = jax.random.split(key, 16)
    nrm = lambda k, shape, s: jax.random.normal(k, shape, jnp.float32) * s
    L, D = DEPTH, D_MODEL
    return {
        "x": nrm(ks[0], (BATCH, SEQ, D), 1.0),
        "c": nrm(ks[1], (BATCH, D), 1.0),
        "ada_w": nrm(ks[2], (L, D, N_MOD * D), 0.5 * D ** -0.5),
        "ada_b": nrm(ks[3], (L, N_MOD * D), 0.02),
        "norm_pre_mix": 1.0 + nrm(ks[4], (L, D), 0.02),
        "norm_post_mix": 1.0 + nrm(ks[5], (L, D), 0.02),
        "w_in": nrm(ks[6], (L, D, IN_COLS), D ** -0.5),
        "attn_sinks": nrm(ks[7], (L, A_Q_HEADS), 0.5),
        "w_o_a": nrm(ks[8], (L, A_Q_W, D), A_Q_W ** -0.5),
        "w_o_b": nrm(ks[9], (L, B_W, D), B_W ** -0.5),
        "w_out": nrm(ks[10], (L, D, D), D ** -0.5),
        "norm_pre_ffn": 1.0 + nrm(ks[11], (L, D), 0.02),
        "norm_post_ffn": 1.0 + nrm(ks[12], (L, D), 0.02),
        "w_gate_up": nrm(ks[13], (L, D, 2 * D_FF), D ** -0.5),
        "w_down": nrm(ks[14], (L, D_FF, D), D_FF ** -0.5),
    }


def reference(x, c, ada_w, ada_b, norm_pre_mix, norm_post_mix, w_in, attn_sinks,
              w_o_a, w_o_b, w_out, norm_pre_ffn, norm_post_ffn, w_gate_up, w_down):
    cond = jax.nn.silu(c)
    for l in range(DEPTH):
        mod = (cond @ ada_w[l] + ada_b[l])[:, None, :]
        sh1, sc1, gt1, sh2, sc2, gt2 = jnp.split(mod, N_MOD, axis=-1)
        h = rms_norm(x, norm_pre_mix[l]) * (1.0 + sc1) + sh1
        y = mixer(h, w_in[l], attn_sinks[l], w_o_a[l], w_o_b[l], w_out[l])
        x = x + gt1 * rms_norm(y, norm_post_mix[l])
        h = rms_norm(x, norm_pre_ffn[l]) * (1.0 + sc2) + sh2
        y = swiglu(h, w_gate_up[l], w_down[l])
        x = x + gt2 * rms_norm(y, norm_post_ffn[l])
    return x
```

```python
from contextlib import ExitStack
import numpy as np
import ml_dtypes
import concourse.bass as bass
import concourse.mybir as mybir
from concourse.bass_utils import run_bass_kernel_spmd

F32 = mybir.dt.float32
BF16 = mybir.dt.bfloat16
ALU = mybir.AluOpType
AF = mybir.ActivationFunctionType
AX = mybir.AxisListType

L = 2
D = 1024
SEQ = 2048
NB = 8
DFF = 2816
NF = DFF // 128
HALF = 1024
BIG = 32768.0
EPS = 1e-6
NEG = -3.0e38

COMPUTE = ("pe", "act", "dve", "pool")
N_LANES = {"sp": 8, "act": 2, "pool": 6}


class Sems:
    def __init__(self, nc, stack):
        self.nc = nc
        self.eng = {e: stack.enter_context(nc.semaphore("s_" + e)) for e in COMPUTE}
        self.eng_cnt = {e: 0 for e in COMPUTE}
        self.lane = {q: [stack.enter_context(nc.semaphore(f"d_{q}{i}")) for i in range(n)]
                     for q, n in N_LANES.items()}
        self.lane_cnt = {q: [0] * n for q, n in N_LANES.items()}
        self.lane_rr = {q: 0 for q in N_LANES}
        self.known = {e: {} for e in ("pe", "act", "dve", "pool", "sp")}


class Phase:
    def __init__(self, nc, sems, name):
        self.nc, self.S, self.name = nc, sems, name
        self.ops = []
        self.last_writer = {}
        self.readers = {}

    def op(self, eng, fn, reads=(), writes=(), dma=False):
        idx = len(self.ops)
        deps = set()
        for r in reads:
            if r in self.last_writer:
                deps.add(self.last_writer[r])
        for w in writes:
            if w in self.last_writer:
                deps.add(self.last_writer[w])
            for rd in self.readers.get(w, ()):
                deps.add(rd)
        for r in reads:
            self.readers.setdefault(r, []).append(idx)
        for w in writes:
            self.last_writer[w] = idx
            self.readers[w] = []
        deps.discard(idx)
        self.ops.append(dict(eng=eng, fn=fn, deps=deps, dma=dma, sig=None, users=0))
        return idx

    def dma(self, queue, out, in_, reads=(), writes=()):
        def fn(e):
            return e.dma_start(out=out, in_=in_)
        return self.op(queue, fn, reads, writes, dma=True)

    def emit(self):
        nc, S, ops = self.nc, self.S, self.ops
        lane_prev = {}
        for i, o in enumerate(ops):
            if o["dma"]:
                q = o["eng"]
                ln = S.lane_rr[q]
                S.lane_rr[q] = (ln + 1) % len(S.lane[q])
                o["lane"] = ln
                if (q, ln) in lane_prev:
                    o["deps"].add(lane_prev[(q, ln)])
                lane_prev[(q, ln)] = i
        for i, o in enumerate(ops):
            for d in o["deps"]:
                p = ops[d]
                if p["eng"] == "pe" and o["eng"] == "pe" and not p["dma"] and not o["dma"]:
                    continue
                p["users"] += 1
        for o in ops:
            if o["dma"]:
                q, ln = o["eng"], o["lane"]
                S.lane_cnt[q][ln] += 16
                o["sig"] = (("lane", q, ln), S.lane[q][ln], S.lane_cnt[q][ln], 16)
            elif o["users"] > 0:
                e = o["eng"]
                S.eng_cnt[e] += 1
                o["sig"] = (("eng", e), S.eng[e], S.eng_cnt[e], 1)
        streams = {}
        for i, o in enumerate(ops):
            streams.setdefault(o["eng"], []).append(i)
        final_dma = {}
        for o in ops:
            if o["dma"]:
                final_dma.setdefault(o["eng"], {})[o["sig"][0]] = o["sig"]

        def make(stream, idxs):
            def body(e):
                known = S.known[stream]
                for i in idxs:
                    o = ops[i]
                    waits = {}
                    for d in o["deps"]:
                        p = ops[d]
                        if p["sig"] is None:
                            continue
                        if p["eng"] == "pe" and stream == "pe" and not p["dma"] and not o["dma"]:
                            continue
                        key, sem, val, _ = p["sig"]
                        if known.get(key, 0) >= val:
                            continue
                        if key not in waits or waits[key][1] < val:
                            waits[key] = (sem, val)
                    for key, (sem, val) in waits.items():
                        e.wait_ge(sem, val)
                        known[key] = val
                    inst = o["fn"](e)
                    if o["sig"] is not None:
                        inst.then_inc(o["sig"][1], o["sig"][3])
                for key, (_, sem, val, _) in final_dma.get(stream, {}).items():
                    if known.get(key, 0) < val:
                        e.wait_ge(sem, val)
                        known[key] = val
            return body

        with nc.Block() as block:
            reg = {"pe": block.tensor, "act": block.scalar, "dve": block.vector,
                   "pool": block.gpsimd, "sp": block.sync}
            for stream, idxs in streams.items():
                reg[stream](make(stream, idxs))


class SbufPlan:
    def __init__(self, nc, base=16512, limit=229344):
        self.nc, self.n, self.cur, self.limit, self.peak = nc, 0, base, limit, base

    def alloc(self, name, shape, dtype):
        esz = 2 if dtype == BF16 else 4
        nbytes = int(np.prod(shape[1:])) * esz
        off = (self.cur + 63) // 64 * 64
        self.cur = off + nbytes
        self.peak = max(self.peak, self.cur)
        if self.cur > self.limit:
            raise RuntimeError(f"SBUF overflow at {name}: {self.cur} > {self.limit}")
        self.n += 1
        return self.nc.alloc_sbuf_tensor_at(f"{name}_{self.n}", list(shape), dtype, offset=off)

    def mark(self):
        return self.cur

    def reset(self, mark):
        self.cur = mark


SM_ADAB = 0
SM_GAIN = L * 48
SM_SINK = SM_GAIN + L * 32
SM_W = SM_SINK + L * 8

N_WIN = 17
N_TAIL = 16
N_GU = 22
N_WD = 16


def build_program(n_layers=L, max_ph=None):
    nc = bass.Bass("TRN2", target_bir_lowering=False)
    pcnt = [0]

    def emit_if(ph):
        if max_ph is None or pcnt[0] < max_ph:
            ph.emit()
        pcnt[0] += 1
    x_d = nc.dram_tensor("xT", [128, 8 * SEQ], F32, kind="ExternalInput").ap()
    c_d = nc.dram_tensor("cT", [128, 8], F32, kind="ExternalInput").ap()
    sm_d = nc.dram_tensor("smalls", [128, SM_W], F32, kind="ExternalInput").ap()
    cst_d = nc.dram_tensor("cst", [128, 384], F32, kind="ExternalInput").ap()
    swam_d = nc.dram_tensor("swam", [128, 4096], F32, kind="ExternalInput").ap()
    alb_d = nc.dram_tensor("alibib", [128, 128], F32, kind="ExternalInput").ap()
    ind_d = nc.dram_tensor("ind", [128, 2048], F32, kind="ExternalInput").ap()
    selc_d = nc.dram_tensor("selc", [2, 2048], F32, kind="ExternalInput").ap()
    ada_d = nc.dram_tensor("ada", [L * 24, 128, 2048], F32, kind="ExternalInput").ap()
    win_d = nc.dram_tensor("win", [L * N_WIN, 128, 2048], F32, kind="ExternalInput").ap()
    wtl_d = nc.dram_tensor("wtail", [L * N_TAIL, 128, 2048], F32, kind="ExternalInput").ap()
    wgu_d = nc.dram_tensor("wgu", [L * N_GU, 128, 2048], F32, kind="ExternalInput").ap()
    wd_d = nc.dram_tensor("wd", [L * N_WD, 128, 1408], F32, kind="ExternalInput").ap()
    y_d = nc.dram_tensor("yT", [128, 8 * SEQ], F32, kind="ExternalOutput").ap()

    with ExitStack() as st:
        S_ = Sems(nc, st)
        pb = [st.enter_context(nc.psum_tensor(f"pb{i}", [128, 512], F32)) for i in range(8)]
        plan = SbufPlan(nc)
        xT = plan.alloc("xT", [128, 8, SEQ], F32)
        cstb = plan.alloc("cstb", [128, 384], BF16)
        ident, cmask, onesb = cstb[:, 0:128], cstb[:, 128:256], cstb[:, 256:384]
        swam = plan.alloc("swam", [128, 8, 2, 2, 128], BF16)
        alb = plan.alloc("alb", [128, 128], F32)
        indt = plan.alloc("indt", [128, 8, 2, 128], BF16)
        smalls = plan.alloc("smalls", [128, SM_W], F32)
        modv = plan.alloc("modv", [128, L * 48], F32)
        vecs = plan.alloc("vecs", [128, L, 6, 8], F32)
        esink = plan.alloc("esink", [128, L * 8], F32)
        epsb = plan.alloc("epsb", [128, 1], F32)
        cTt = plan.alloc("cTt", [128, 8], F32)
        cond = plan.alloc("cond", [128, 8], BF16)
        pmark = plan.mark()

        def bank3(i, a, b):
            return pb[i][:, 0:a * b].rearrange("p (a b) -> p a b", a=a)

        ph = Phase(nc, S_, "init")
        for c in range(8):
            ph.dma("sp", xT[:, c, :], x_d[:, c * SEQ:(c + 1) * SEQ], writes=[("xT", c)])
        ph.dma("sp", smalls[:], sm_d, writes=["smalls"])
        ph.dma("sp", cTt[:], c_d, writes=["cTt"])
        ph.dma("sp", alb[:], alb_d, writes=["alb"])
        ph.dma("pool", cstb[:], cst_d, writes=["cstb"])
        swam_flat = swam[:].rearrange("p a b c d -> p (a b c d)")
        ph.dma("pool", swam_flat[:, 0:2048], swam_d[:, 0:2048], writes=["swam0"])
        ph.dma("pool", swam_flat[:, 2048:4096], swam_d[:, 2048:4096], writes=["swam1"])
        ph.dma("pool", indt[:].rearrange("p a j b -> p (a j b)"), ind_d, writes=["indt"])
        ph.op("dve", lambda e: e.memset(epsb[:], EPS), writes=["epsb"])
        ph.op("act", lambda e: e.activation(cond[:], cTt[:], AF.Silu), reads=["cTt"], writes=["cond"])
        ph.op("act", lambda e: e.activation(esink[:], smalls[:, SM_SINK:SM_SINK + L * 8], AF.Exp),
              reads=["smalls"], writes=["esink"])
        abuf = [plan.alloc(f"abuf{i}", [128, 2, 8, 128], BF16) for i in range(3)]
        for pc in range(24):
            b = pc % 3
            ph.dma("pool", abuf[b][:].rearrange("p a k n -> p (a k n)"), ada_d[pc], writes=[("abuf", b)])

            def mm(e, pc=pc, b=b):
                r = None
                for jj in range(2):
                    j = pc * 2 + jj
                    for k in range(8):
                        r = e.matmul(pb[0][:, j:j + 1], abuf[b][:, jj, k, :], cond[:, k:k + 1],
                                     start=(k == 0), stop=(k == 7))
                return r
            ph.op("pe", mm, reads=[("abuf", b), "cond"], writes=["pb0"])
        nm = 48
        ph.op("dve", lambda e: e.tensor_tensor(modv[:, 0:nm], pb[0][:, 0:nm], smalls[:, SM_ADAB:SM_ADAB + nm], ALU.add),
              reads=["pb0", "smalls"], writes=["modv"])

        def derive_vecs(ph, l):
            mo = l * 48
            g = lambda gi, l=l: smalls[:, SM_GAIN + l * 32 + gi * 8: SM_GAIN + l * 32 + gi * 8 + 8]
            m = lambda mi, mo=mo: modv[:, mo + mi * 8: mo + mi * 8 + 8]
            ph.op("dve", lambda e, l=l, m=m, g=g: e.scalar_tensor_tensor(vecs[:, l, 0, :], m(1), 1.0, g(0), ALU.add, ALU.mult),
                  reads=["modv", "smalls"], writes=[("vecs", l, 0)])
            ph.op("dve", lambda e, l=l, m=m: e.tensor_copy(vecs[:, l, 1, :], m(0)), reads=["modv"], writes=[("vecs", l, 1)])
            ph.op("dve", lambda e, l=l, m=m, g=g: e.scalar_tensor_tensor(vecs[:, l, 2, :], m(2), 0.5, g(1), ALU.mult, ALU.mult),
                  reads=["modv", "smalls"], writes=[("vecs", l, 2)])
            ph.op("dve", lambda e, l=l, m=m, g=g: e.scalar_tensor_tensor(vecs[:, l, 3, :], m(4), 1.0, g(2), ALU.add, ALU.mult),
                  reads=["modv", "smalls"], writes=[("vecs", l, 3)])
            ph.op("dve", lambda e, l=l, m=m: e.tensor_copy(vecs[:, l, 4, :], m(3)), reads=["modv"], writes=[("vecs", l, 4)])
            ph.op("dve", lambda e, l=l, m=m, g=g: e.tensor_tensor(vecs[:, l, 5, :], m(5), g(3), ALU.mult),
                  reads=["modv", "smalls"], writes=[("vecs", l, 5)])
        derive_vecs(ph, 0)
        ph.emit()
        plan.reset(pmark)

        def stream(n, nbuf, issue, compute):
            for i in range(min(nbuf - 1, n)):
                issue(i)
            for i in range(n):
                if i + nbuf - 1 < n:
                    issue(i + nbuf - 1)
                compute(i)

        def hkeys(s):
            return [("hT", s, c) for c in range(8)]

        def prenorm1(ph, cg, s, sqs, sdt, rstd, ssbank):
            t0 = cg * 512
            for c in range(8):
                sq = sqs[c % 2]
                ph.op("act", lambda e, c=c, sq=sq: e.activation(sq[:], xT[:, c, t0:t0 + 512], AF.Square),
                      reads=[], writes=[("sqs", c % 2)])
                ph.op("pe", lambda e, c=c, sq=sq: e.matmul(pb[ssbank][:], onesb, sq[:], start=(c == 0), stop=(c == 7)),
                      reads=[("sqs", c % 2)], writes=[("pb", ssbank)])
            ph.op("act", lambda e: e.activation(sdt[0][:], pb[ssbank][:], AF.Sqrt, bias=epsb[:, 0:1], scale=1.0 / D),
                  reads=[("pb", ssbank)], writes=["sdt"])
            ph.op("dve", lambda e: e.reciprocal(rstd[s][:], sdt[0][:]), reads=["sdt"], writes=[("rstd", s)])

        def prenorm2(ph, l, va, vb, hT, cg, s, rstd, tmp):
            t0 = cg * 512
            for c in range(8):
                tb = tmp[c % 2]
                ph.op("dve", lambda e, c=c, tb=tb: e.scalar_tensor_tensor(
                    tb[:], xT[:, c, t0:t0 + 512], vecs[:, l, va, c:c + 1], rstd[s][:], ALU.mult, ALU.mult),
                    reads=[("rstd", s)], writes=[("tmp", c % 2)])
                ph.op("act", lambda e, c=c, tb=tb: e.activation(
                    hT[:, c, s * 512:(s + 1) * 512], tb[:], AF.Identity, bias=vecs[:, l, vb, c:c + 1], scale=1.0),
                    reads=[("tmp", c % 2)], writes=[("hT", s, c)])

        def prenorm_both(ph, l, va, vb, hT, u, sqs, sdt, rstd, tmp):
            prenorm1(ph, 2 * u, 0, sqs, sdt, rstd, 0)
            prenorm2(ph, l, va, vb, hT, 2 * u, 0, rstd, tmp)
            prenorm1(ph, 2 * u + 1, 1, sqs, sdt, rstd, 7)
            prenorm2(ph, l, va, vb, hT, 2 * u + 1, 1, rstd, tmp)

        def postnorm(ph, l, vc, y, cg, sdt, rstd, tmp, ssbank, msdiv, store=False):
            t0 = cg * 512
            ph.op("act", lambda e: e.activation(sdt[:], pb[ssbank][:], AF.Sqrt, bias=epsb[:, 0:1], scale=1.0 / msdiv),
                  reads=[("pb", ssbank)], writes=["sdt"])
            ph.op("dve", lambda e: e.reciprocal(rstd[:], sdt[:]), reads=["sdt"], writes=["rstd"])
            for c in range(8):
                tb = tmp[c % len(tmp)]
                ph.op("pool", lambda e, c=c, tb=tb: e.tensor_tensor(tb[:], y[:, c, :], rstd[:], ALU.mult),
                      reads=[("y", c), "rstd"], writes=[("tmp", c % len(tmp))])
                ph.op("dve", lambda e, c=c, tb=tb: e.scalar_tensor_tensor(
                    xT[:, c, t0:t0 + 512], tb[:], vecs[:, l, vc, c:c + 1], xT[:, c, t0:t0 + 512], ALU.mult, ALU.add),
                    reads=[("tmp", c % len(tmp))], writes=[("xTw", cg, c)])
                if store:
                    ph.dma("sp", y_d[:, c * SEQ + t0: c * SEQ + t0 + 512], xT[:, c, t0:t0 + 512],
                           reads=[("xTw", cg, c)], writes=[("yd", cg, c)])

        def ydown(ph, mmfn, wreads, y, sq8, l, vc, cg, sdt, rstd, tmp, msdiv, rot0, store=False, hook=None):
            for j in range(8):
                bank = (rot0 + j) % 4
                ph.op("pe", lambda e, j=j, bank=bank: mmfn(e, j, bank),
                      reads=(wreads(j) if callable(wreads) else wreads), writes=[("pb", bank)])
                ph.op("dve", lambda e, j=j, bank=bank: e.tensor_copy(y[:, j, :], pb[bank][:]),
                      reads=[("pb", bank)], writes=[("y", j)])
                ph.op("act", lambda e, j=j: e.activation(sq8[:, j, :], y[:, j, :], AF.Square),
                      reads=[("y", j)], writes=[("sq8", j)])
                if hook is not None:
                    hook(j)

            def mmss(e):
                r = None
                for j in range(8):
                    r = e.matmul(pb[4][:], onesb, sq8[:, j, :], start=(j == 0), stop=(j == 7))
                return r
            ph.op("pe", mmss, reads=[("sq8", j) for j in range(8)], writes=[("pb", 4)])
            postnorm(ph, l, vc, y, cg, sdt, rstd, tmp, 4, msdiv, store)

        K16 = 16384
        for l in range(n_layers):
            plan.reset(pmark)
            kaT = plan.alloc("kaT", [128, SEQ], BF16)
            kbT = plan.alloc("kbT", [128, 4, SEQ], BF16)
            Va = plan.alloc("Va", [128, 16, 2, 65], BF16)
            Vb = plan.alloc("Vb", [128, 16, 8, 65], BF16)
            kmf = plan.alloc("kmf", [128, 4, 8], F32)
            kmT = plan.alloc("kmT", [128, 4, 8], BF16)
            lmark = (plan.mark() + 63) // 64 * 64
            for u in range(2):
                plan.reset(lmark)
                hT = plan.alloc("hT", [128, 8, HALF], BF16)
                oT = plan.alloc("oT", [128, 8, HALF], BF16)
                qpa = plan.alloc("qpa", [128, 8, HALF], BF16)
                qpb = plan.alloc("qpb", [128, 8, HALF], BF16)
                assert plan.mark() == lmark + 4 * K16
                ph = Phase(nc, S_, f"A{l}{u}")
                sqs = [plan.alloc(f"sqs{i}", [128, 512], BF16) for i in range(2)]
                sdt = [plan.alloc(f"sdt{i}", [128, 512], F32) for i in range(1)]
                rstd = [plan.alloc(f"rstd{i}", [128, 512], F32) for i in range(2)]
                tmp = [plan.alloc(f"tmp{i}", [128, 512], F32) for i in range(2)]
                wbuf = [plan.alloc(f"wbuf{i}", [128, 2, 8, 128], BF16) for i in range(3)]
                def memsetsA():
                    ph.op("pool", lambda e: e.memset(qpa[64:128, 0:4, :], 0.0), writes=["qpa"])
                    ph.op("pool", lambda e: e.memset(qpa[0:64, 4:8, :], 0.0), writes=["qpa"])
                    for hh in range(8):
                        lo = 64 if hh % 2 == 0 else 0
                        ph.op("pool", lambda e, hh=hh, lo=lo: e.memset(qpb[lo:lo + 64, hh, :], 0.0), writes=["qpb"])
                    if u == 0:
                        ph.op("pool", lambda e: e.memset(Va[:], 1.0), writes=["Va"])
                        ph.op("pool", lambda e: e.memset(Vb[:], 1.0), writes=["Vb"])
                def needA(s):
                    pass
                kinds = [("aq", 0), ("aq", 1), ("aq", 2), ("aq", 3), ("ak", 0), ("av", 0),
                         ("bq", 0), ("bq", 1), ("bq", 2), ("bq", 3), ("bk", 0), ("bk", 1), ("bk", 2), ("bk", 3),
                         ("bv", 0), ("bv", 1), ("bv", 2), ("bv", 3)]
                rots = dict(p=0, v=0)

                def issueA(pc):
                    b = pc % 3
                    ph.dma("pool", wbuf[b][:].rearrange("p a k n -> p (a k n)"), win_d[l * N_WIN + pc],
                           writes=[("wbuf", b)])

                def computeA(pc):
                    b = pc % 3
                    if pc == 0:
                        memsetsA()
                        prenorm_both(ph, l, 0, 1, hT, u, sqs, sdt, rstd, tmp)
                    for jj in range(2):
                        kind, ci = kinds[pc * 2 + jj]
                        if kind in ("aq", "ak", "bq", "bk"):
                            for s in range(2):
                                needA(s)
                                bank = 1 + rots["p"] % 4
                                rots["p"] += 1

                                def mm(e, b=b, jj=jj, s=s, bank=bank):
                                    r = None
                                    for k in range(8):
                                        r = e.matmul(pb[bank][:], wbuf[b][:, jj, k, :], hT[:, k, s * 512:(s + 1) * 512],
                                                     start=(k == 0), stop=(k == 7))
                                    return r
                                ph.op("pe", mm, reads=[("wbuf", b)] + hkeys(s), writes=[("pb", bank)])
                                cs = slice(s * 512, (s + 1) * 512)
                                gs = slice(u * HALF + s * 512, u * HALF + (s + 1) * 512)
                                if kind == "aq":
                                    ph.op("act", lambda e, bank=bank, ci=ci, cs=cs: e.activation(
                                        qpa[0:64, ci, cs], pb[bank][0:64, :], AF.Copy),
                                        reads=[("pb", bank), "qpa"], writes=[("qpa_d", ci, s)])
                                    ph.op("dve", lambda e, bank=bank, ci=ci, cs=cs: e.tensor_copy(
                                        qpa[64:128, 4 + ci, cs], pb[bank][64:128, :]),
                                        reads=[("pb", bank), "qpa"], writes=[("qpa_d", 4 + ci, s)])
                                elif kind == "bq":
                                    ph.op("act", lambda e, bank=bank, ci=ci, cs=cs: e.activation(
                                        qpb[0:64, 2 * ci, cs], pb[bank][0:64, :], AF.Copy),
                                        reads=[("pb", bank), "qpb"], writes=[("qpb_d", 2 * ci, s)])
                                    ph.op("dve", lambda e, bank=bank, ci=ci, cs=cs: e.tensor_copy(
                                        qpb[64:128, 2 * ci + 1, cs], pb[bank][64:128, :]),
                                        reads=[("pb", bank), "qpb"], writes=[("qpb_d", 2 * ci + 1, s)])
                                elif kind == "ak":
                                    ph.op("act", lambda e, bank=bank, gs=gs: e.activation(kaT[:, gs], pb[bank][:], AF.Copy),
                                          reads=[("pb", bank)], writes=[("kaT", s)])
                                else:
                                    ph.op("dve", lambda e, bank=bank, gs=gs, ci=ci: e.tensor_copy(kbT[:, ci, gs], pb[bank][:]),
                                          reads=[("pb", bank)], writes=[("kbT", ci, s)])
                            if kind == "bk":
                                nb0 = u * 4
                                ph.op("dve", lambda e, ci=ci, nb0=nb0: e.tensor_reduce(
                                    kmf[:, ci, nb0:nb0 + 4],
                                    kbT[:, ci, u * HALF:(u + 1) * HALF].rearrange("p (n t) -> p n t", n=4),
                                    AX.X, ALU.add),
                                    reads=[("kbT", ci, 0), ("kbT", ci, 1)], writes=[("kmf", ci)])
                                ph.op("dve", lambda e, ci=ci, nb0=nb0: e.tensor_scalar(
                                    kmT[:, ci, nb0:nb0 + 4], kmf[:, ci, nb0:nb0 + 4], 1.0 / 256.0, None, ALU.mult),
                                    reads=[("kmf", ci)], writes=[("kmT", ci)])
                        else:
                            for t4 in range(2):
                                bank = 5 + rots["v"] % 2
                                rots["v"] += 1

                                def mm(e, b=b, jj=jj, t4=t4, bank=bank):
                                    r = None
                                    for tt in range(4):
                                        tok = (t4 * 4 + tt) * 128
                                        for k in range(8):
                                            r = e.matmul(pb[bank][:, tt * 128:(tt + 1) * 128], hT[:, k, tok:tok + 128],
                                                         wbuf[b][:, jj, k, :], start=(k == 0), stop=(k == 7))
                                    return r
                                ph.op("pe", mm, reads=[("wbuf", b)] + hkeys(t4), writes=[("pb", bank)])
                                gt0 = u * 8 + t4 * 4
                                src = pb[bank][:].rearrange("p (t g d) -> p t g d", t=4, g=2)
                                if kind == "av":
                                    ph.op("act", lambda e, src=src, gt0=gt0: e.activation(
                                        Va[:, gt0:gt0 + 4, :, 0:64], src, AF.Copy),
                                        reads=[("pb", bank), "Va"], writes=[("Va_d", t4)])
                                else:
                                    ph.op("dve", lambda e, src=src, gt0=gt0, ci=ci: e.tensor_copy(
                                        Vb[:, gt0:gt0 + 4, 2 * ci:2 * ci + 2, 0:64], src),
                                        reads=[("pb", bank), "Vb"], writes=[("Vb_d", ci, t4)])
                stream(9, 3, issueA, computeA)
                emit_if(ph)

                plan.reset(lmark + 4 * K16)
                ph = Phase(nc, S_, f"B{l}{u}")
                gate = plan.alloc("gate", [128, 2, 8, 8], F32)
                top8 = plan.alloc("top8", [128, 2, 8, 8], F32)
                selb = plan.alloc("selb", [128, 2, 8, 8], BF16)
                selT = [plan.alloc(f"selT{i}", [128, 8, 256], BF16) for i in range(2)]
                pT = [plan.alloc(f"pT{i}", [128, 2, 256], BF16) for i in range(3)]
                pTa = [plan.alloc(f"pTa{i}", [128, 4, 128], BF16) for i in range(3)]
                otok = [plan.alloc(f"otok{i}", [128, 16, 64], BF16) for i in range(4)]
                den = [plan.alloc(f"den{i}", [128, 4, 1], F32) for i in range(4)]
                R = dict(s=0, o=0, p=0, pa=0, d=0)
                items = []
                if u == 1:
                    for i in range(2):
                        ph.op("pool", lambda e, i=i: e.memset(selT[i][:], 0.0), writes=[("selT", i, 0), ("selT", i, 1)])
                        ph.dma("pool", selT[i][8:10, :, :].rearrange("p h t -> p (h t)"), selc_d,
                               reads=[("selT", i, 0), ("selT", i, 1)], writes=[("selTc", i)])

                def normalize(br, g, jq, ob, ot, okey):
                    dn = den[R["d"] % 4]
                    dkey = ("den", R["d"] % 4)
                    R["d"] += 1
                    o3 = pb[ob][:, 0:260].rearrange("p (h d) -> p h d", h=4)
                    if br == "a":
                        ph.op("dve", lambda e: e.tensor_tensor(
                            dn[:], o3[:, :, 64:65], esink[:, l * 8 + 4 * g: l * 8 + 4 * g + 4].unsqueeze(2), ALU.add),
                            reads=[("pb", ob)], writes=[dkey])
                        ph.op("dve", lambda e: e.reciprocal(dn[:], dn[:]), reads=[dkey], writes=[dkey])
                    else:
                        ph.op("dve", lambda e: e.reciprocal(dn[:], o3[:, :, 64:65]), reads=[("pb", ob)], writes=[dkey])
                    h0 = (0 if br == "a" else 8) + 4 * g
                    ph.op("dve", lambda e: e.tensor_tensor(
                        ot[:, h0:h0 + 4, :], o3[:, :, 0:64], dn[:].broadcast_to([128, 4, 64]), ALU.mult),
                        reads=[("pb", ob), dkey], writes=[okey + (br, g)])

                def gating1(qn):
                    ownn = 4 * u + qn
                    tqn = qn * 256
                    ph.op("pool", lambda e: e.memset(gate[:], NEG), writes=["gate"])

                    def mmg(e):
                        r = None
                        for tt in range(2):
                            for h in range(8):
                                r = e.matmul(pb[2][:, (tt * 8 + h) * 8:(tt * 8 + h) * 8 + 8],
                                             qpb[:, h, tqn + tt * 128: tqn + tt * 128 + 128], kmT[:, h // 2, :],
                                             start=True, stop=True)
                        return r
                    ph.op("pe", mmg, reads=[], writes=["pb2"])
                    gsrc = pb[2][:, 0:128].rearrange("p (t h n) -> p t h n", t=2, h=8)
                    ph.op("dve", lambda e: e.tensor_copy(gate[:, :, :, 0:ownn], gsrc[:, :, :, 0:ownn]),
                          reads=["pb2", "gate"], writes=["gate"])
                    for tt in range(2):
                        for h in range(8):
                            ph.op("dve", lambda e, tt=tt, h=h: e.max(top8[:, tt, h, :], gate[:, tt, h, :]),
                                  reads=["gate"], writes=[("top8", tt, h)])
                            ph.op("dve", lambda e, tt=tt, h=h: e.tensor_scalar(
                                selb[:, tt, h, :], gate[:, tt, h, :], top8[:, tt, h, 2:3], 1.0, ALU.is_ge, ALU.subtract),
                                reads=["gate", ("top8", tt, h)], writes=[("selb", tt, h)])

                def gating3(qn):
                    st_ = selT[qn % 2]
                    for hg in range(2):
                        def tr(e, hg=hg):
                            r = None
                            dst = pb[2][:].bitcast(BF16).rearrange("p (h t) -> p h t", h=4)
                            for hh in range(4):
                                for tt in range(2):
                                    r = e.transpose(dst[0:8, hh, tt * 128:(tt + 1) * 128], selb[:, tt, hg * 4 + hh, :], ident)
                            return r
                        ph.op("pe", tr, reads=[("selb", tt, h) for tt in range(2) for h in range(hg * 4, hg * 4 + 4)],
                              writes=["pb2"])
                        srcT = pb[2][:].bitcast(BF16).rearrange("p (h t) -> p h t", h=4)
                        ph.op("dve", lambda e, hg=hg, srcT=srcT: e.tensor_copy(
                            st_[0:8, hg * 4:hg * 4 + 4, :], srcT[0:8, :, :]),
                            reads=["pb2"], writes=[("selT", qn % 2, hg)])

                def otranspose(qn):
                    for jq in range(2):
                        ot = otok[(qn % 2) * 2 + jq]
                        okey = ("otok", (qn % 2) * 2 + jq)

                        def trn(e, ot=ot):
                            r = None
                            dst = pb[2][:].bitcast(BF16).rearrange("p (c t) -> p c t", c=8)
                            for c in range(8):
                                r = e.transpose(dst[:, c, :], ot[:, 2 * c:2 * c + 2, :].rearrange("p a b -> p (a b)"), ident)
                            return r
                        ph.op("pe", trn, reads=[okey + (br, g) for br in ("a", "b") for g in range(2)], writes=["pb2"])
                        srcT = pb[2][:].bitcast(BF16).rearrange("p (c t) -> p c t", c=8)
                        tcol = qn * 256 + jq * 128
                        ph.op("dve", lambda e, srcT=srcT, tcol=tcol: e.tensor_copy(oT[:, :, tcol:tcol + 128], srcT),
                              reads=["pb2"], writes=[("oT", qn, jq)])

                for qb in range(4):
                    own = 4 * u + qb
                    tq0 = qb * 256
                    gating = own >= 4
                    selq = selT[qb % 2]
                    first_item_of_qb = len(items)
                    pre_ops = []
                    if gating and qb == 0:
                        pre_ops.append(lambda qb=qb: (gating1(qb), gating3(qb)))
                    nxt_gating = (qb + 1 < 4) and (4 * u + qb + 1 >= 4)
                    if nxt_gating:
                        pre_ops.append(lambda qb=qb: gating1(qb + 1))
                    for g in range(2):
                        obs = []
                        for jq in range(2):
                            obs.append(4 + R["o"] % 4)
                            R["o"] += 1
                        for jq in range(2):
                            ob = obs[jq]
                            i_glob = 2 * own + jq
                            ktiles = ([i_glob - 1] if i_glob > 0 else []) + [i_glob]
                            tqc = slice(tq0 + jq * 128, tq0 + jq * 128 + 128)
                            for ki, kt in enumerate(ktiles):
                                typ = 0 if kt == i_glob else 1
                                sbank = (0, 1, 3)[R["s"] % 3]
                                R["s"] += 1
                                pa = pTa[R["pa"] % 3]
                                pakey = ("pTa", R["pa"] % 3)
                                R["pa"] += 1

                                def S(g=g, kt=kt, typ=typ, sbank=sbank, tqc=tqc):
                                    def mms(e):
                                        r = None
                                        for hh in range(4):
                                            h = 4 * g + hh
                                            dst = pb[sbank][:, hh * 128:(hh + 1) * 128]
                                            e.matmul(dst, kaT[:, kt * 128:(kt + 1) * 128], qpa[:, h, tqc], start=True, stop=False)
                                            e.matmul(dst, ident, swam[:, h, typ, 0, :], start=False, stop=False)
                                            r = e.matmul(dst, ident, swam[:, h, typ, 1, :], start=False, stop=True)
                                        return r
                                    ph.op("pe", mms, reads=[], writes=[("sb", sbank)])

                                def E(sbank=sbank, pa=pa, pakey=pakey):
                                    ph.op("act", lambda e: e.activation(
                                        pa[:].rearrange("p a b -> p (a b)"), pb[sbank][:], AF.Exp, scale=0.125),
                                        reads=[("sb", sbank)], writes=[pakey])

                                def PV(g=g, kt=kt, pa=pa, pakey=pakey, ob=ob, first=(ki == 0), last=(ki == len(ktiles) - 1)):
                                    def mmpv(e):
                                        r = None
                                        for hh in range(4):
                                            r = e.matmul(pb[ob][:, hh * 65:hh * 65 + 65], pa[:, hh, :], Va[:, kt, g, :],
                                                         start=(first and hh == 0), stop=last, skip_group_check=True)
                                        return r
                                    ph.op("pe", mmpv, reads=[pakey], writes=[("pb", ob)])
                                items.append(dict(S=S, E=E, PV=PV, pre=[], post=[]))
                        post = items[-1]["post"]
                        for jq in range(2):
                            post.append(lambda g=g, jq=jq, ob=obs[jq], qb=qb: normalize(
                                "a", g, jq, ob, otok[(qb % 2) * 2 + jq], ("otok", (qb % 2) * 2 + jq)))
                        if g == 0:
                            if nxt_gating:
                                post.append(lambda qb=qb: gating3(qb + 1))
                            if qb > 0:
                                post.append(lambda qb=qb: otranspose(qb - 1))
                    for hg in range(2):
                        obs = []
                        for jq in range(2):
                            obs.append(4 + R["o"] % 4)
                            R["o"] += 1
                        for hh in range(4):
                            h = hg * 4 + hh
                            if gating:
                                for n in range(own):
                                    dist = own - n
                                    si = (0, 1, 3)[R["s"] % 3]
                                    R["s"] += 1
                                    pt = pT[R["p"] % 3]
                                    ptkey = ("pT", R["p"] % 3)
                                    R["p"] += 1

                                    def S2(h=h, n=n, si=si, tq0=tq0, selq=selq, qb=qb):
                                        def mmq(e):
                                            r = None
                                            for j in range(2):
                                                dst = pb[si][:, j * 256:(j + 1) * 256]
                                                kt = 2 * n + j
                                                e.matmul(dst, kbT[:, h // 2, kt * 128:(kt + 1) * 128], qpb[:, h, tq0:tq0 + 256],
                                                         start=True, stop=False)
                                                r = e.matmul(dst, indt[:, n, j, :], selq[:, h, :], start=False, stop=True)
                                            return r
                                        ph.op("pe", mmq, reads=[("selT", qb % 2, h // 4)], writes=[("sb", si)])

                                    def E2(si=si, pt=pt, ptkey=ptkey, bcol=h * 16 + dist * 2):
                                        ph.op("act", lambda e: e.activation(
                                            pt[:].rearrange("p j t -> p (j t)"), pb[si][:], AF.Exp,
                                            bias=alb[:, bcol:bcol + 1], scale=0.125),
                                            reads=[("sb", si)], writes=[ptkey])

                                    def PV2(h=h, hh=hh, n=n, pt=pt, ptkey=ptkey, obs=obs):
                                        def mmpv(e):
                                            r = None
                                            for j in range(2):
                                                for jq in range(2):
                                                    r = e.matmul(pb[obs[jq]][:, hh * 65:hh * 65 + 65],
                                                                 pt[:, j, jq * 128:(jq + 1) * 128], Vb[:, 2 * n + j, h, :],
                                                                 start=(n == 0 and j == 0), stop=False)
                                            return r
                                        ph.op("pe", mmpv, reads=[ptkey], writes=[("pb", obs[0]), ("pb", obs[1])])
                                    items.append(dict(S=S2, E=E2, PV=PV2, pre=[], post=[]))
                                kts = [(own, 0), (own, 1)]
                            else:
                                kts = [(n, j) for n in range(own) for j in range(2)] + [(own, 0), (own, 1)]
                            for (n, j) in kts:
                                dist = own - n
                                kt = n * 2 + j
                                si = (0, 1, 3)[R["s"] % 3]
                                R["s"] += 1
                                ps_s = pb[si][:, 0:256]
                                pt = pT[R["p"] % 3]
                                ptkey = ("pT", R["p"] % 3)
                                R["p"] += 1
                                c0 = 128 if (n == own and j == 1) else 0

                                def S(h=h, n=n, j=j, kt=kt, ps_s=ps_s, own=own, tq0=tq0, gating=gating, si=si, selq=selq, qb=qb):
                                    def mmq(e):
                                        lhs = kbT[:, h // 2, kt * 128:(kt + 1) * 128]
                                        if n < own:
                                            r = e.matmul(ps_s, lhs, qpb[:, h, tq0:tq0 + 256], start=True, stop=not gating)
                                            if gating:
                                                r = e.matmul(ps_s, indt[:, n, 0, :], selq[:, h, :], start=False, stop=True)
                                        else:
                                            d0 = j * 128
                                            e.matmul(ps_s[:, d0:d0 + 128], lhs, qpb[:, h, tq0 + d0:tq0 + d0 + 128],
                                                     start=True, stop=False)
                                            r = e.matmul(ps_s[:, d0:d0 + 128], ident, cmask, start=False, stop=True)
                                            if j == 0:
                                                r = e.matmul(ps_s[:, 128:256], lhs, qpb[:, h, tq0 + 128:tq0 + 256],
                                                             start=True, stop=True)
                                        return r
                                    rd = [("selT", qb % 2, h // 4)] if (gating and n < own) else []
                                    ph.op("pe", mmq, reads=rd, writes=[("sb", si)])

                                def E(ps_s=ps_s, pt=pt, ptkey=ptkey, c0=c0, si=si, bcol=h * 16 + dist * 2 + j):
                                    ph.op("act", lambda e: e.activation(
                                        pt[:, 0, c0:256], ps_s[:, c0:256], AF.Exp, bias=alb[:, bcol:bcol + 1], scale=0.125),
                                        reads=[("sb", si)], writes=[ptkey])

                                def PV(h=h, hh=hh, kt=kt, pt=pt, ptkey=ptkey, obs=obs, n=n, j=j, own=own):
                                    def mmpv(e):
                                        r = None
                                        first = (kt == 0)
                                        for jq in range(2):
                                            if n == own and j == 1 and jq == 0:
                                                continue
                                            last = (n == own and j == jq)
                                            r = e.matmul(pb[obs[jq]][:, hh * 65:hh * 65 + 65], pt[:, 0, jq * 128:(jq + 1) * 128],
                                                         Vb[:, kt, h, :], start=first, stop=last)
                                        return r
                                    ph.op("pe", mmpv, reads=[ptkey], writes=[("pb", obs[0]), ("pb", obs[1])])
                                items.append(dict(S=S, E=E, PV=PV, pre=[], post=[]))
                        post = items[-1]["post"]
                        for jq in range(2):
                            post.append(lambda hg=hg, jq=jq, ob=obs[jq], qb=qb: normalize(
                                "b", hg, jq, ob, otok[(qb % 2) * 2 + jq], ("otok", (qb % 2) * 2 + jq)))
                    items[first_item_of_qb]["pre"] = pre_ops
                items[-1]["post"].append(lambda: otranspose(3))
                LA = 2
                for idx in range(len(items) + LA):
                    if idx < len(items):
                        for f in items[idx]["pre"]:
                            f()
                        items[idx]["S"]()
                        items[idx]["E"]()
                    k = idx - LA
                    if k >= 0:
                        items[k]["PV"]()
                        for f in items[k]["post"]:
                            f()
                emit_if(ph)

                plan.reset(lmark + 2 * K16)
                ph = Phase(nc, S_, f"C1{l}{u}")
                merged = plan.alloc("merged", [128, 8, HALF], BF16)
                wA = [plan.alloc(f"wA{i}", [128, 2, 8, 128], BF16) for i in range(2)]
                wB = [plan.alloc(f"wB{i}", [128, 2, 2, 4, 128], BF16) for i in range(2)]
                tg = [plan.alloc(f"tg{i}", [128, 512], F32) for i in range(4)]
                mt = [plan.alloc(f"mt{i}", [128, 512], F32) for i in range(4)]
                R = dict(r=0)

                def issueC1(j):
                    ab = j % 2
                    ph.dma("pool", wA[ab][:].rearrange("p a k n -> p (a k n)"), wtl_d[l * N_TAIL + j], writes=[("wA", ab)])
                    if j % 2 == 0:
                        bb = (j // 2) % 2
                        ph.dma("pool", wB[bb][:].rearrange("p a b k n -> p (a b k n)"), wtl_d[l * N_TAIL + 8 + j // 2],
                               writes=[("wB", bb)])

                def computeC1(j):
                    ab, bb, jp = j % 2, (j // 2) % 2, j % 2
                    for s in range(2):
                        r4 = R["r"] % 2
                        R["r"] += 1
                        bga, bgb, boa, bob = 4 * r4, 4 * r4 + 1, 4 * r4 + 2, 4 * r4 + 3
                        cs = slice(s * 512, (s + 1) * 512)

                        def mm(e, ab=ab, bb=bb, jp=jp, cs=cs, bga=bga, bgb=bgb, boa=boa, bob=bob):
                            r = None
                            for k in range(8):
                                e.matmul(pb[bga][:], wA[ab][:, 0, k, :], hT[:, k, cs], start=(k == 0), stop=(k == 7))
                            for k in range(8):
                                e.matmul(pb[bgb][:], wA[ab][:, 1, k, :], hT[:, k, cs], start=(k == 0), stop=(k == 7))
                            for k in range(4):
                                e.matmul(pb[boa][:], wB[bb][:, jp, 0, k, :], oT[:, k, cs], start=(k == 0), stop=(k == 3))
                            for k in range(4):
                                r = e.matmul(pb[bob][:], wB[bb][:, jp, 1, k, :], oT[:, 4 + k, cs], start=(k == 0), stop=(k == 3))
                            return r
                        ph.op("pe", mm, reads=[("wA", ab), ("wB", bb)], writes=[("pbg", r4)])
                        ta, tb2 = tg[2 * r4], tg[2 * r4 + 1]
                        m1, m2 = mt[2 * r4], mt[2 * r4 + 1]
                        ph.op("act", lambda e, ta=ta, bga=bga: e.activation(ta[:], pb[bga][:], AF.Tanh, scale=0.5),
                              reads=[("pbg", r4)], writes=[("tg", 2 * r4)])
                        ph.op("act", lambda e, tb2=tb2, bgb=bgb: e.activation(tb2[:], pb[bgb][:], AF.Tanh, scale=0.5),
                              reads=[("pbg", r4)], writes=[("tg", 2 * r4 + 1)])
                        ph.op("dve", lambda e, ta=ta, m1=m1, boa=boa: e.scalar_tensor_tensor(
                            m1[:], ta[:], 1.0, pb[boa][:], ALU.add, ALU.mult),
                            reads=[("tg", 2 * r4), ("pbg", r4)], writes=[("mt", 2 * r4)])
                        ph.op("dve", lambda e, tb2=tb2, m2=m2, bob=bob: e.scalar_tensor_tensor(
                            m2[:], tb2[:], 1.0, pb[bob][:], ALU.add, ALU.mult),
                            reads=[("tg", 2 * r4 + 1), ("pbg", r4)], writes=[("mt", 2 * r4 + 1)])
                        ph.op("dve", lambda e, m1=m1, m2=m2, j=j, cs=cs: e.tensor_tensor(merged[:, j, cs], m1[:], m2[:], ALU.add),
                              reads=[("mt", 2 * r4), ("mt", 2 * r4 + 1)], writes=[("merged", j, s)])
                stream(8, 2, issueC1, computeC1)
                emit_if(ph)

                plan.reset(lmark)
                ph = Phase(nc, S_, f"C2{l}{u}")
                wout = plan.alloc("wout", [128, 4, 2, 8, 128], BF16)
                y = plan.alloc("y", [128, 8, 512], F32)
                plan.reset(lmark + 3 * K16)
                sq2 = plan.alloc("sq8", [128, 8, 512], BF16)
                sdt = plan.alloc("sdt", [128, 512], F32)
                rstd = plan.alloc("rstd", [128, 512], F32)
                tmp = [plan.alloc(f"tmp{i}", [128, 512], F32) for i in range(4)]
                for pp in range(4):
                    ph.dma("pool", wout[:, pp].rearrange("p a k n -> p (a k n)"), wtl_d[l * N_TAIL + 12 + pp],
                           writes=[("wout", pp)])
                nxt = l + 1 < n_layers
                if nxt:
                    abuf2 = [plan.alloc(f"abuf2{i}", [128, 2, 8, 128], BF16) for i in range(3)]

                    def ada_issue(i):
                        ph.dma("pool", abuf2[i % 3][:].rearrange("p a k n -> p (a k n)"),
                               ada_d[(l + 1) * 24 + u * 12 + i], writes=[("abuf2", i % 3)])

                    def ada_mm(i):
                        def mm(e, i=i):
                            r = None
                            for jj in range(2):
                                col = i * 2 + jj
                                for k in range(8):
                                    r = e.matmul(pb[5][:, col:col + 1], abuf2[i % 3][:, jj, k, :], cond[:, k:k + 1],
                                                 start=(k == 0), stop=(k == 7))
                            return r
                        ph.op("pe", mm, reads=[("abuf2", i % 3)], writes=["pb5"])
                    for i in range(3):
                        ada_issue(i)
                for s in range(2):
                    cs = slice(s * 512, (s + 1) * 512)

                    def mmf(e, j, bank, cs=cs):
                        r = None
                        for k in range(8):
                            r = e.matmul(pb[bank][:], wout[:, j // 2, j % 2, k, :], merged[:, k, cs],
                                         start=(k == 0), stop=(k == 7))
                        return r

                    def hook(j, s=s):
                        i = s * 8 + j
                        if nxt and i < 12:
                            ada_mm(i)
                            if i + 3 < 12:
                                ada_issue(i + 3)
                    ydown(ph, mmf, (lambda j: [("wout", j // 2)]), y, sq2, l, 2, 2 * u + s, sdt, rstd, tmp, 4.0 * D, 0,
                          hook=hook)
                if nxt:
                    c0 = (l + 1) * 48 + u * 24
                    ph.op("dve", lambda e, c0=c0: e.tensor_tensor(
                        modv[:, c0:c0 + 24], pb[5][:, 0:24], smalls[:, SM_ADAB + c0:SM_ADAB + c0 + 24], ALU.add),
                        reads=["pb5"], writes=["modv"])
                    if u == 1:
                        derive_vecs(ph, l + 1)
                emit_if(ph)
            for u in range(2):
                plan.reset(pmark)
                actT = plan.alloc("actT", [128, NF, HALF], BF16)
                fmark = plan.mark()
                ph = Phase(nc, S_, f"F1{l}{u}")
                h2T = plan.alloc("h2T", [128, 8, HALF], BF16)
                sqs = [plan.alloc(f"sqs{i}", [128, 512], BF16) for i in range(2)]
                sdt = [plan.alloc(f"sdt{i}", [128, 512], F32) for i in range(1)]
                rstd = [plan.alloc(f"rstd{i}", [128, 512], F32) for i in range(2)]
                tmp = [plan.alloc(f"tmp{i}", [128, 512], F32) for i in range(2)]
                wg = [plan.alloc(f"wg{i}", [128, 2, 8, 128], BF16) for i in range(3)]
                sg = [plan.alloc(f"sg{i}", [128, 512], F32) for i in range(3)]
                R = dict(r=0)

                def issueF1(f):
                    b = f % 3
                    ph.dma("pool", wg[b][:].rearrange("p a k n -> p (a k n)"), wgu_d[l * N_GU + f], writes=[("wg", b)])

                def computeF1(f):
                    b = f % 3
                    if f == 0:
                        prenorm_both(ph, l, 3, 4, h2T, u, sqs, sdt, rstd, tmp)
                    for s in range(2):
                        r3 = R["r"] % 3
                        R["r"] += 1
                        bg, bu = 1 + 2 * r3, 2 + 2 * r3
                        cs = slice(s * 512, (s + 1) * 512)

                        def mm(e, b=b, cs=cs, bg=bg, bu=bu):
                            r = None
                            for k in range(8):
                                e.matmul(pb[bg][:], wg[b][:, 0, k, :], h2T[:, k, cs], start=(k == 0), stop=(k == 7))
                            for k in range(8):
                                r = e.matmul(pb[bu][:], wg[b][:, 1, k, :], h2T[:, k, cs], start=(k == 0), stop=(k == 7))
                            return r
                        ph.op("pe", mm, reads=[("wg", b)] + hkeys(s), writes=[("pbp", r3)])
                        sgt = sg[r3]
                        ph.op("act", lambda e, sgt=sgt, bg=bg: e.activation(sgt[:], pb[bg][:], AF.Silu),
                              reads=[("pbp", r3)], writes=[("sg", r3)])
                        ph.op("dve", lambda e, sgt=sgt, bu=bu, f=f, cs=cs: e.tensor_tensor(actT[:, f, cs], sgt[:], pb[bu][:], ALU.mult),
                              reads=[("sg", r3), ("pbp", r3)], writes=[("actT", f, s)])
                stream(NF, 3, issueF1, computeF1)
                emit_if(ph)
                plan.reset(fmark)
                ph = Phase(nc, S_, f"F2{l}{u}")
                wdn = plan.alloc("wdn", [128, 8, NF, 128], BF16)
                y = plan.alloc("y", [128, 8, 512], F32)
                sq2 = plan.alloc("sq8", [128, 8, 512], BF16)
                sdt = plan.alloc("sdt", [128, 512], F32)
                rstd = plan.alloc("rstd", [128, 512], F32)
                tmp = [plan.alloc(f"tmp{i}", [128, 512], F32) for i in range(4)]
                for pp in range(N_WD):
                    ph.dma("pool", wdn[:, pp // 2, (pp % 2) * 11:(pp % 2) * 11 + 11, :].rearrange("p f n -> p (f n)"),
                           wd_d[l * N_WD + pp], writes=[("wdn", pp)])
                for s in range(2):
                    cs = slice(s * 512, (s + 1) * 512)

                    def mmf(e, j, bank, cs=cs):
                        r = None
                        for f in range(NF):
                            r = e.matmul(pb[bank][:], wdn[:, j, f, :], actT[:, f, cs],
                                         start=(f == 0), stop=(f == NF - 1))
                        return r
                    ydown(ph, mmf, (lambda j: [("wdn", 2 * j), ("wdn", 2 * j + 1)]), y, sq2, l, 5, 2 * u + s, sdt, rstd, tmp, 1.0 * D, 0,
                          store=(l == n_layers - 1 and max_ph is None))
                emit_if(ph)
        if max_ph is not None:
            ph = Phase(nc, S_, "out")
            for c in range(8):
                ph.dma("sp", y_d[:, c * SEQ:(c + 1) * SEQ], xT[:, c, :], reads=[], writes=[("y_d", c)])
            ph.emit()
    return nc


def _kmaj(W, cols=None):
    if cols is not None:
        W = W[:, cols]
    K, N = W.shape
    return np.ascontiguousarray(W.reshape(K // 128, 128, N).transpose(1, 0, 2))


def _consts():
    i = np.arange(1, 17, dtype=np.float32)
    sl = (2.0 ** (-8.0 * i / 16)).astype(np.float32)
    sa, sb = sl[:8], sl[8:]
    p = np.arange(128)[:, None]
    f = np.arange(128)[None, :]
    ident = (p == f).astype(np.float32)
    cmask = np.where(p <= f, 0.0, -BIG).astype(np.float32)
    ones = np.ones((128, 128), np.float32)
    cst = np.concatenate([ident, cmask, ones], axis=1)
    swam = np.zeros((128, 8, 2, 2, 128), np.float32)
    for h in range(8):
        for typ in range(2):
            if typ == 0:
                valid = p <= f
                dist = (f - p).astype(np.float64)
            else:
                valid = p > f
                dist = (128 + f - p).astype(np.float64)
            M = np.where(valid, -8.0 * float(sa[h]) * dist, -BIG)
            hi = M.astype(np.float32).astype(ml_dtypes.bfloat16).astype(np.float32)
            lo = (M - hi).astype(np.float32).astype(ml_dtypes.bfloat16).astype(np.float32)
            swam[:, h, typ, 0, :] = hi
            swam[:, h, typ, 1, :] = lo
    alb = np.zeros((128, 128), np.float32)
    for h in range(8):
        for dist in range(8):
            for j in range(2):
                alb[:, h * 16 + dist * 2 + j] = sb[h] * (np.arange(128) + 128 * j - 256 * dist)
    ind = np.zeros((128, 8, 2, 128), np.float32)
    for n in range(8):
        ind[n, n, :, :] = BIG
    ind[8, :, 1, :] = 1.0
    ind[9, :, 1, :] = 1.0
    selc = np.zeros((2, 8, 256), np.float32)
    for h in range(8):
        v = 1024.0 * float(sb[h])
        hi = np.float32(v).astype(ml_dtypes.bfloat16).astype(np.float32)
        lo = np.float32(v - hi).astype(ml_dtypes.bfloat16).astype(np.float32)
        selc[0, h, :] = hi
        selc[1, h, :] = lo
    return cst, swam.reshape(128, 4096), alb, ind.reshape(128, 2048), selc.reshape(2, 2048)


def _prep_weights(ada_w, ada_b, norm_pre_mix, norm_post_mix, w_in, attn_sinks, w_o_a, w_o_b, w_out,
                  norm_pre_ffn, norm_post_ffn, w_gate_up, w_down):
    f32 = lambda a: np.asarray(a, dtype=np.float32)
    ada_w, w_in, w_o_a, w_o_b, w_out, w_gate_up, w_down = map(f32, (ada_w, w_in, w_o_a, w_o_b, w_out, w_gate_up, w_down))
    sm = np.zeros((128, SM_W), np.float32)
    for l in range(L):
        sm[:, SM_ADAB + l * 48: SM_ADAB + (l + 1) * 48] = f32(ada_b[l]).reshape(48, 128).T
        for gi, gn in enumerate((norm_pre_mix, norm_post_mix, norm_pre_ffn, norm_post_ffn)):
            sm[:, SM_GAIN + l * 32 + gi * 8: SM_GAIN + l * 32 + gi * 8 + 8] = f32(gn[l]).reshape(8, 128).T
        sm[:, SM_SINK + l * 8: SM_SINK + l * 8 + 8] = f32(attn_sinks[l])[None, :]
    ada = np.zeros((L * 24, 128, 2, 8, 128), np.float32)
    for l in range(L):
        A = _kmaj(ada_w[l]).reshape(128, 8, 48, 128)
        A = A.transpose(2, 0, 1, 3)
        ada[l * 24:(l + 1) * 24] = A.reshape(24, 2, 128, 8, 128).transpose(0, 2, 1, 3, 4)
    ada = ada.reshape(L * 24, 128, 2048)
    off = np.cumsum([0, 512, 128, 128, 512, 512, 512, 1024, 1024])
    o_aq, o_ak, o_av, o_bq, o_bk, o_bv, o_ga, o_gb = off[:8]
    chunks = []
    for c in range(4):
        chunks.append(np.concatenate([o_aq + c * 64 + np.arange(64), o_aq + (4 + c) * 64 + np.arange(64)]))
    chunks.append(o_ak + np.arange(128))
    chunks.append(o_av + np.arange(128))
    for c in range(4):
        chunks.append(o_bq + c * 128 + np.arange(128))
    for c in range(4):
        chunks.append(o_bk + c * 128 + np.arange(128))
    for c in range(4):
        chunks.append(o_bv + c * 128 + np.arange(128))
    win = np.zeros((L * N_WIN, 128, 2, 8, 128), np.float32)
    wtl = np.zeros((L * N_TAIL, 128, 2048), np.float32)
    wgu = np.zeros((L * N_GU, 128, 2, 8, 128), np.float32)
    wdd = np.zeros((L * N_WD, 128, 11, 128), np.float32)
    for l in range(L):
        for ci, cols in enumerate(chunks):
            win[l * N_WIN + ci // 2, :, ci % 2] = _kmaj(w_in[l], cols)
        for j in range(8):
            t = np.zeros((128, 2, 8, 128), np.float32)
            t[:, 0] = _kmaj(w_in[l], o_ga + j * 128 + np.arange(128))
            t[:, 1] = _kmaj(w_in[l], o_gb + j * 128 + np.arange(128))
            wtl[l * N_TAIL + j] = t.reshape(128, 2048)
        for jj in range(4):
            t = np.zeros((128, 2, 2, 4, 128), np.float32)
            for jp in range(2):
                j = 2 * jj + jp
                t[:, jp, 0] = _kmaj(w_o_a[l], j * 128 + np.arange(128))
                t[:, jp, 1] = _kmaj(w_o_b[l], j * 128 + np.arange(128))
            wtl[l * N_TAIL + 8 + jj] = t.reshape(128, 2048)
        for pp in range(4):
            t = np.zeros((128, 2, 8, 128), np.float32)
            for jp in range(2):
                t[:, jp] = _kmaj(w_out[l], (2 * pp + jp) * 128 + np.arange(128))
            wtl[l * N_TAIL + 12 + pp] = t.reshape(128, 2048)
        for f in range(NF):
            wgu[l * N_GU + f, :, 0] = _kmaj(w_gate_up[l], f * 128 + np.arange(128))
            wgu[l * N_GU + f, :, 1] = _kmaj(w_gate_up[l], DFF + f * 128 + np.arange(128))
        W4 = w_down[l].reshape(2, 11, 128, 8, 128)
        wdd[l * N_WD:(l + 1) * N_WD] = W4.transpose(3, 0, 2, 1, 4).reshape(16, 128, 11, 128)
    cst, swam, alb, ind, selc = _consts()
    return dict(smalls=sm, ada=ada, win=win.reshape(L * N_WIN, 128, 2048), wtail=wtl,
                wgu=wgu.reshape(L * N_GU, 128, 2048), wd=wdd.reshape(L * N_WD, 128, 1408),
                cst=cst, swam=swam, alibib=alb, ind=ind, selc=selc)


def make_in_maps(x, c, **w):
    shared = _prep_weights(**w)
    x = np.asarray(x, dtype=np.float32)
    c = np.asarray(c, dtype=np.float32)
    maps = []
    for b in range(x.shape[0]):
        xT = np.ascontiguousarray(x[b].T.reshape(8, 128, SEQ).transpose(1, 0, 2)).reshape(128, 8 * SEQ)
        cT = np.ascontiguousarray(c[b].reshape(8, 128).T)
        m = dict(shared)
        m["xT"] = xT
        m["cT"] = cT
        maps.append(m)
    return maps


def unpack_out(yT):
    return np.ascontiguousarray(yT.reshape(128, 8, SEQ).transpose(2, 1, 0).reshape(SEQ, D))


_NC_CACHE = {}


def kernel(x, c, ada_w, ada_b, norm_pre_mix, norm_post_mix, w_in, attn_sinks, w_o_a, w_o_b, w_out,
           norm_pre_ffn, norm_post_ffn, w_gate_up, w_down):
    maps = make_in_maps(x, c, ada_w=ada_w, ada_b=ada_b, norm_pre_mix=norm_pre_mix, norm_post_mix=norm_post_mix,
                        w_in=w_in, attn_sinks=attn_sinks, w_o_a=w_o_a, w_o_b=w_o_b, w_out=w_out,
                        norm_pre_ffn=norm_pre_ffn, norm_post_ffn=norm_post_ffn, w_gate_up=w_gate_up, w_down=w_down)
    if "nc" not in _NC_CACHE:
        _NC_CACHE["nc"] = build_program(L)
    nc = _NC_CACHE["nc"]
    res = run_bass_kernel_spmd(nc, maps, core_ids=list(range(8)))
    out = np.stack([unpack_out(np.asarray(r["yT"])) for r in res.results], axis=0)
    return out.astype(np.float32)
```

```python
from contextlib import ExitStack
import numpy as np
import ml_dtypes
import concourse.bass as bass
import concourse.mybir as mybir
from concourse.bass_utils import run_bass_kernel_spmd

F32 = mybir.dt.float32
BF16 = mybir.dt.bfloat16
ALU = mybir.AluOpType
AF = mybir.ActivationFunctionType
AX = mybir.AxisListType

L = 2
D = 1024
SEQ = 2048
NB = 8
DFF = 2816
NF = DFF // 128
HALF = 1024
BIG = 32768.0
EPS = 1e-6
NEG = -3.0e38

COMPUTE = ("pe", "act", "dve", "pool")
N_LANES = {"sp": 8, "act": 2, "pool": 6}


class Sems:
    def __init__(self, nc, stack):
        self.nc = nc
        self.eng = {e: stack.enter_context(nc.semaphore("s_" + e)) for e in COMPUTE}
        self.eng_cnt = {e: 0 for e in COMPUTE}
        self.lane = {q: [stack.enter_context(nc.semaphore(f"d_{q}{i}")) for i in range(n)]
                     for q, n in N_LANES.items()}
        self.lane_cnt = {q: [0] * n for q, n in N_LANES.items()}
        self.lane_rr = {q: 0 for q in N_LANES}
        self.known = {e: {} for e in ("pe", "act", "dve", "pool", "sp")}


class Phase:
    def __init__(self, nc, sems, name):
        self.nc, self.S, self.name = nc, sems, name
        self.ops = []
        self.last_writer = {}
        self.readers = {}

    def op(self, eng, fn, reads=(), writes=(), dma=False):
        idx = len(self.ops)
        deps = set()
        for r in reads:
            if r in self.last_writer:
                deps.add(self.last_writer[r])
        for w in writes:
            if w in self.last_writer:
                deps.add(self.last_writer[w])
            for rd in self.readers.get(w, ()):
                deps.add(rd)
        for r in reads:
            self.readers.setdefault(r, []).append(idx)
        for w in writes:
            self.last_writer[w] = idx
            self.readers[w] = []
        deps.discard(idx)
        self.ops.append(dict(eng=eng, fn=fn, deps=deps, dma=dma, sig=None, users=0))
        return idx

    def dma(self, queue, out, in_, reads=(), writes=()):
        def fn(e):
            return e.dma_start(out=out, in_=in_)
        return self.op(queue, fn, reads, writes, dma=True)

    def emit(self):
        nc, S, ops = self.nc, self.S, self.ops
        lane_prev = {}
        for i, o in enumerate(ops):
            if o["dma"]:
                q = o["eng"]
                ln = S.lane_rr[q]
                S.lane_rr[q] = (ln + 1) % len(S.lane[q])
                o["lane"] = ln
                if (q, ln) in lane_prev:
                    o["deps"].add(lane_prev[(q, ln)])
                lane_prev[(q, ln)] = i
        for i, o in enumerate(ops):
            for d in o["deps"]:
                p = ops[d]
                if p["eng"] == "pe" and o["eng"] == "pe" and not p["dma"] and not o["dma"]:
                    continue
                p["users"] += 1
        for o in ops:
            if o["dma"]:
                q, ln = o["eng"], o["lane"]
                S.lane_cnt[q][ln] += 16
                o["sig"] = (("lane", q, ln), S.lane[q][ln], S.lane_cnt[q][ln], 16)
            elif o["users"] > 0:
                e = o["eng"]
                S.eng_cnt[e] += 1
                o["sig"] = (("eng", e), S.eng[e], S.eng_cnt[e], 1)
        streams = {}
        for i, o in enumerate(ops):
            streams.setdefault(o["eng"], []).append(i)
        final_dma = {}
        for o in ops:
            if o["dma"]:
                final_dma.setdefault(o["eng"], {})[o["sig"][0]] = o["sig"]

        def make(stream, idxs):
            def body(e):
                known = S.known[stream]
                for i in idxs:
                    o = ops[i]
                    waits = {}
                    for d in o["deps"]:
                        p = ops[d]
                        if p["sig"] is None:
                            continue
                        if p["eng"] == "pe" and stream == "pe" and not p["dma"] and not o["dma"]:
                            continue
                        key, sem, val, _ = p["sig"]
                        if known.get(key, 0) >= val:
                            continue
                        if key not in waits or waits[key][1] < val:
                            waits[key] = (sem, val)
                    for key, (sem, val) in waits.items():
                        e.wait_ge(sem, val)
                        known[key] = val
                    inst = o["fn"](e)
                    if o["sig"] is not None:
                        inst.then_inc(o["sig"][1], o["sig"][3])
                for key, (_, sem, val, _) in final_dma.get(stream, {}).items():
                    if known.get(key, 0) < val:
                        e.wait_ge(sem, val)
                        known[key] = val
            return body

        with nc.Block(no_gpsimd_drain=True) as block:
            reg = {"pe": block.tensor, "act": block.scalar, "dve": block.vector,
                   "pool": block.gpsimd, "sp": block.sync}
            for stream, idxs in streams.items():
                reg[stream](make(stream, idxs))


class SbufPlan:
    def __init__(self, nc, base=16512, limit=229344):
        self.nc, self.n, self.cur, self.limit, self.peak = nc, 0, base, limit, base

    def alloc(self, name, shape, dtype):
        esz = 2 if dtype == BF16 else 4
        nbytes = int(np.prod(shape[1:])) * esz
        off = (self.cur + 63) // 64 * 64
        self.cur = off + nbytes
        self.peak = max(self.peak, self.cur)
        if self.cur > self.limit:
            raise RuntimeError(f"SBUF overflow at {name}: {self.cur} > {self.limit}")
        self.n += 1
        return self.nc.alloc_sbuf_tensor_at(f"{name}_{self.n}", list(shape), dtype, offset=off)

    def mark(self):
        return self.cur

    def reset(self, mark):
        self.cur = mark


SM_ADAB = 0
SM_GAIN = L * 48
SM_SINK = SM_GAIN + L * 32
SM_W = SM_SINK + L * 8

N_WIN = 17
N_TAIL = 16
N_GU = 22
N_WD = 16


def build_program(n_layers=L, max_ph=None):
    nc = bass.Bass("TRN2", target_bir_lowering=False)
    pcnt = [0]

    def emit_if(ph):
        if max_ph is None or pcnt[0] < max_ph:
            ph.emit()
        pcnt[0] += 1
    x_d = nc.dram_tensor("xT", [128, 8 * SEQ], F32, kind="ExternalInput").ap()
    c_d = nc.dram_tensor("cT", [128, 8], F32, kind="ExternalInput").ap()
    sm_d = nc.dram_tensor("smalls", [128, SM_W], F32, kind="ExternalInput").ap()
    cst_d = nc.dram_tensor("cst", [128, 384], F32, kind="ExternalInput").ap()
    swam_d = nc.dram_tensor("swam", [128, 4096], F32, kind="ExternalInput").ap()
    alb_d = nc.dram_tensor("alibib", [128, 128], F32, kind="ExternalInput").ap()
    ind_d = nc.dram_tensor("ind", [128, 2048], F32, kind="ExternalInput").ap()
    selc_d = nc.dram_tensor("selc", [2, 2048], F32, kind="ExternalInput").ap()
    ada_d = nc.dram_tensor("ada", [L * 24, 128, 2048], F32, kind="ExternalInput").ap()
    win_d = nc.dram_tensor("win", [L * N_WIN, 128, 2048], F32, kind="ExternalInput").ap()
    wtl_d = nc.dram_tensor("wtail", [L * N_TAIL, 128, 2048], F32, kind="ExternalInput").ap()
    wgu_d = nc.dram_tensor("wgu", [L * N_GU, 128, 2048], F32, kind="ExternalInput").ap()
    wd_d = nc.dram_tensor("wd", [L * N_WD, 128, 1408], F32, kind="ExternalInput").ap()
    y_d = nc.dram_tensor("yT", [128, 8 * SEQ], F32, kind="ExternalOutput").ap()

    with ExitStack() as st:
        S_ = Sems(nc, st)
        pb = [st.enter_context(nc.psum_tensor(f"pb{i}", [128, 512], F32)) for i in range(8)]
        plan = SbufPlan(nc)
        xT = plan.alloc("xT", [128, 8, SEQ], F32)
        cstb = plan.alloc("cstb", [128, 384], BF16)
        ident, cmask, onesb = cstb[:, 0:128], cstb[:, 128:256], cstb[:, 256:384]
        swam = plan.alloc("swam", [128, 8, 2, 2, 128], BF16)
        alb = plan.alloc("alb", [128, 128], F32)
        indt = plan.alloc("indt", [128, 8, 2, 128], BF16)
        smalls = plan.alloc("smalls", [128, SM_W], F32)
        modv = plan.alloc("modv", [128, L * 48], F32)
        vecs = plan.alloc("vecs", [128, L, 6, 8], F32)
        esink = plan.alloc("esink", [128, L * 8], F32)
        epsb = plan.alloc("epsb", [128, 1], F32)
        cTt = plan.alloc("cTt", [128, 8], F32)
        cond = plan.alloc("cond", [128, 8], BF16)
        pmark = plan.mark()

        def bank3(i, a, b):
            return pb[i][:, 0:a * b].rearrange("p (a b) -> p a b", a=a)

        ph = Phase(nc, S_, "init")
        for c in range(8):
            ph.dma("sp", xT[:, c, :], x_d[:, c * SEQ:(c + 1) * SEQ], writes=[("xT", c)])
        ph.dma("sp", smalls[:], sm_d, writes=["smalls"])
        ph.dma("sp", cTt[:], c_d, writes=["cTt"])
        ph.dma("sp", alb[:], alb_d, writes=["alb"])
        ph.dma("pool", cstb[:], cst_d, writes=["cstb"])
        swam_flat = swam[:].rearrange("p a b c d -> p (a b c d)")
        ph.dma("pool", swam_flat[:, 0:2048], swam_d[:, 0:2048], writes=["swam0"])
        ph.dma("pool", swam_flat[:, 2048:4096], swam_d[:, 2048:4096], writes=["swam1"])
        ph.dma("pool", indt[:].rearrange("p a j b -> p (a j b)"), ind_d, writes=["indt"])
        ph.op("dve", lambda e: e.memset(epsb[:], EPS), writes=["epsb"])
        ph.op("act", lambda e: e.activation(cond[:], cTt[:], AF.Silu), reads=["cTt"], writes=["cond"])
        ph.op("act", lambda e: e.activation(esink[:], smalls[:, SM_SINK:SM_SINK + L * 8], AF.Exp),
              reads=["smalls"], writes=["esink"])
        abuf = [plan.alloc(f"abuf{i}", [128, 2, 8, 128], BF16) for i in range(3)]
        for pc in range(24):
            b = pc % 3
            ph.dma("pool", abuf[b][:].rearrange("p a k n -> p (a k n)"), ada_d[pc], writes=[("abuf", b)])

            def mm(e, pc=pc, b=b):
                r = None
                for jj in range(2):
                    j = pc * 2 + jj
                    for k in range(8):
                        r = e.matmul(pb[0][:, j:j + 1], abuf[b][:, jj, k, :], cond[:, k:k + 1],
                                     start=(k == 0), stop=(k == 7))
                return r
            ph.op("pe", mm, reads=[("abuf", b), "cond"], writes=["pb0"])
        nm = 48
        ph.op("dve", lambda e: e.tensor_tensor(modv[:, 0:nm], pb[0][:, 0:nm], smalls[:, SM_ADAB:SM_ADAB + nm], ALU.add),
              reads=["pb0", "smalls"], writes=["modv"])

        def derive_vecs(ph, l):
            mo = l * 48
            g = lambda gi, l=l: smalls[:, SM_GAIN + l * 32 + gi * 8: SM_GAIN + l * 32 + gi * 8 + 8]
            m = lambda mi, mo=mo: modv[:, mo + mi * 8: mo + mi * 8 + 8]
            ph.op("dve", lambda e, l=l, m=m, g=g: e.scalar_tensor_tensor(vecs[:, l, 0, :], m(1), 1.0, g(0), ALU.add, ALU.mult),
                  reads=["modv", "smalls"], writes=[("vecs", l, 0)])
            ph.op("dve", lambda e, l=l, m=m: e.tensor_copy(vecs[:, l, 1, :], m(0)), reads=["modv"], writes=[("vecs", l, 1)])
            ph.op("dve", lambda e, l=l, m=m, g=g: e.scalar_tensor_tensor(vecs[:, l, 2, :], m(2), 0.5, g(1), ALU.mult, ALU.mult),
                  reads=["modv", "smalls"], writes=[("vecs", l, 2)])
            ph.op("dve", lambda e, l=l, m=m, g=g: e.scalar_tensor_tensor(vecs[:, l, 3, :], m(4), 1.0, g(2), ALU.add, ALU.mult),
                  reads=["modv", "smalls"], writes=[("vecs", l, 3)])
            ph.op("dve", lambda e, l=l, m=m: e.tensor_copy(vecs[:, l, 4, :], m(3)), reads=["modv"], writes=[("vecs", l, 4)])
            ph.op("dve", lambda e, l=l, m=m, g=g: e.tensor_tensor(vecs[:, l, 5, :], m(5), g(3), ALU.mult),
                  reads=["modv", "smalls"], writes=[("vecs", l, 5)])
        derive_vecs(ph, 0)
        ph.emit()
        plan.reset(pmark)

        def stream(n, nbuf, issue, compute):
            for i in range(min(nbuf - 1, n)):
                issue(i)
            for i in range(n):
                if i + nbuf - 1 < n:
                    issue(i + nbuf - 1)
                compute(i)

        def hkeys(s):
            return [("hT", s, c) for c in range(8)]

        def prenorm1(ph, cg, s, sqs, sdt, rstd, ssbank):
            t0 = cg * 512
            for c in range(8):
                sq = sqs[c % 2]
                ph.op("act", lambda e, c=c, sq=sq: e.activation(sq[:], xT[:, c, t0:t0 + 512], AF.Square),
                      reads=[], writes=[("sqs", c % 2)])
                ph.op("pe", lambda e, c=c, sq=sq: e.matmul(pb[ssbank][:], onesb, sq[:], start=(c == 0), stop=(c == 7)),
                      reads=[("sqs", c % 2)], writes=[("pb", ssbank)])
            ph.op("act", lambda e: e.activation(sdt[0][:], pb[ssbank][:], AF.Sqrt, bias=epsb[:, 0:1], scale=1.0 / D),
                  reads=[("pb", ssbank)], writes=["sdt"])
            ph.op("dve", lambda e: e.reciprocal(rstd[s][:], sdt[0][:]), reads=["sdt"], writes=[("rstd", s)])

        def prenorm2(ph, l, va, vb, hT, cg, s, rstd, tmp):
            t0 = cg * 512
            for c in range(8):
                tb = tmp[c % 2]
                ph.op("dve", lambda e, c=c, tb=tb: e.scalar_tensor_tensor(
                    tb[:], xT[:, c, t0:t0 + 512], vecs[:, l, va, c:c + 1], rstd[s][:], ALU.mult, ALU.mult),
                    reads=[("rstd", s)], writes=[("tmp", c % 2)])
                ph.op("act", lambda e, c=c, tb=tb: e.activation(
                    hT[:, c, s * 512:(s + 1) * 512], tb[:], AF.Identity, bias=vecs[:, l, vb, c:c + 1], scale=1.0),
                    reads=[("tmp", c % 2)], writes=[("hT", s, c)])

        def prenorm_both(ph, l, va, vb, hT, u, sqs, sdt, rstd, tmp):
            prenorm1(ph, 2 * u, 0, sqs, sdt, rstd, 0)
            prenorm1(ph, 2 * u + 1, 1, sqs, sdt, rstd, 7)
            prenorm2(ph, l, va, vb, hT, 2 * u, 0, rstd, tmp)
            prenorm2(ph, l, va, vb, hT, 2 * u + 1, 1, rstd, tmp)

        def postnorm(ph, l, vc, y, cg, sdt, rstd, tmp, ssbank, msdiv, store=False):
            t0 = cg * 512
            ph.op("act", lambda e: e.activation(sdt[:], pb[ssbank][:], AF.Sqrt, bias=epsb[:, 0:1], scale=1.0 / msdiv),
                  reads=[("pb", ssbank)], writes=["sdt"])
            ph.op("dve", lambda e: e.reciprocal(rstd[:], sdt[:]), reads=["sdt"], writes=["rstd"])
            for c in range(8):
                tb = tmp[c % len(tmp)]
                ph.op("pool", lambda e, c=c, tb=tb: e.tensor_tensor(tb[:], y[:, c, :], rstd[:], ALU.mult),
                      reads=[("y", c), "rstd"], writes=[("tmp", c % len(tmp))])
                ph.op("dve", lambda e, c=c, tb=tb: e.scalar_tensor_tensor(
                    xT[:, c, t0:t0 + 512], tb[:], vecs[:, l, vc, c:c + 1], xT[:, c, t0:t0 + 512], ALU.mult, ALU.add),
                    reads=[("tmp", c % len(tmp))], writes=[("xTw", cg, c)])
                if store:
                    ph.dma("sp", y_d[:, c * SEQ + t0: c * SEQ + t0 + 512], xT[:, c, t0:t0 + 512],
                           reads=[("xTw", cg, c)], writes=[("yd", cg, c)])

        def ydown(ph, mmfn, wreads, y, sq8, l, vc, cg, sdt, rstd, tmp, msdiv, rot0, store=False, hook=None):
            for j in range(8):
                bank = (rot0 + j) % 4
                ph.op("pe", lambda e, j=j, bank=bank: mmfn(e, j, bank),
                      reads=(wreads(j) if callable(wreads) else wreads), writes=[("pb", bank)])
                ph.op("dve", lambda e, j=j, bank=bank: e.tensor_copy(y[:, j, :], pb[bank][:]),
                      reads=[("pb", bank)], writes=[("y", j)])
                ph.op("act", lambda e, j=j: e.activation(sq8[:, j, :], y[:, j, :], AF.Square),
                      reads=[("y", j)], writes=[("sq8", j)])
                if hook is not None:
                    hook(j)

            def mmss(e):
                r = None
                for j in range(8):
                    r = e.matmul(pb[4][:], onesb, sq8[:, j, :], start=(j == 0), stop=(j == 7))
                return r
            ph.op("pe", mmss, reads=[("sq8", j) for j in range(8)], writes=[("pb", 4)])
            postnorm(ph, l, vc, y, cg, sdt, rstd, tmp, 4, msdiv, store)

        K16 = 16384
        for l in range(n_layers):
            plan.reset(pmark)
            kaT = plan.alloc("kaT", [128, SEQ], BF16)
            kbT = plan.alloc("kbT", [128, 4, SEQ], BF16)
            Va = plan.alloc("Va", [128, 16, 2, 65], BF16)
            Vb = plan.alloc("Vb", [128, 16, 8, 65], BF16)
            kmf = plan.alloc("kmf", [128, 4, 8], F32)
            kmT = plan.alloc("kmT", [128, 4, 8], BF16)
            lmark = (plan.mark() + 63) // 64 * 64
            for u in range(2):
                plan.reset(lmark)
                hT = plan.alloc("hT", [128, 8, HALF], BF16)
                oT = plan.alloc("oT", [128, 8, HALF], BF16)
                qpa = plan.alloc("qpa", [128, 8, HALF], BF16)
                qpb = plan.alloc("qpb", [128, 8, HALF], BF16)
                assert plan.mark() == lmark + 4 * K16
                ph = Phase(nc, S_, f"A{l}{u}")
                sqs = [plan.alloc(f"sqs{i}", [128, 512], BF16) for i in range(2)]
                sdt = [plan.alloc(f"sdt{i}", [128, 512], F32) for i in range(1)]
                rstd = [plan.alloc(f"rstd{i}", [128, 512], F32) for i in range(2)]
                tmp = [plan.alloc(f"tmp{i}", [128, 512], F32) for i in range(2)]
                wbuf = [plan.alloc(f"wbuf{i}", [128, 2, 8, 128], BF16) for i in range(3)]
                def memsetsA():
                    ph.op("pool", lambda e: e.memset(qpa[64:128, 0:4, :], 0.0), writes=["qpa"])
                    ph.op("pool", lambda e: e.memset(qpa[0:64, 4:8, :], 0.0), writes=["qpa"])
                    for hh in range(8):
                        lo = 64 if hh % 2 == 0 else 0
                        ph.op("pool", lambda e, hh=hh, lo=lo: e.memset(qpb[lo:lo + 64, hh, :], 0.0), writes=["qpb"])
                    if u == 0:
                        ph.op("pool", lambda e: e.memset(Va[:], 1.0), writes=["Va"])
                        ph.op("pool", lambda e: e.memset(Vb[:], 1.0), writes=["Vb"])
                def needA(s):
                    pass
                kinds = [("aq", 0), ("aq", 1), ("aq", 2), ("aq", 3), ("ak", 0), ("av", 0),
                         ("bq", 0), ("bq", 1), ("bq", 2), ("bq", 3), ("bk", 0), ("bk", 1), ("bk", 2), ("bk", 3),
                         ("bv", 0), ("bv", 1), ("bv", 2), ("bv", 3)]
                rots = dict(p=0, v=0)

                def issueA(pc):
                    b = pc % 3
                    ph.dma("pool", wbuf[b][:].rearrange("p a k n -> p (a k n)"), win_d[l * N_WIN + pc],
                           writes=[("wbuf", b)])

                def computeA(pc):
                    b = pc % 3
                    if pc == 0:
                        memsetsA()
                        prenorm_both(ph, l, 0, 1, hT, u, sqs, sdt, rstd, tmp)
                    for jj in range(2):
                        kind, ci = kinds[pc * 2 + jj]
                        if kind in ("aq", "ak", "bq", "bk"):
                            for s in range(2):
                                needA(s)
                                bank = 1 + rots["p"] % 4
                                rots["p"] += 1

                                def mm(e, b=b, jj=jj, s=s, bank=bank):
                                    r = None
                                    for k in range(8):
                                        r = e.matmul(pb[bank][:], wbuf[b][:, jj, k, :], hT[:, k, s * 512:(s + 1) * 512],
                                                     start=(k == 0), stop=(k == 7))
                                    return r
                                ph.op("pe", mm, reads=[("wbuf", b)] + hkeys(s), writes=[("pb", bank)])
                                cs = slice(s * 512, (s + 1) * 512)
                                gs = slice(u * HALF + s * 512, u * HALF + (s + 1) * 512)
                                if kind == "aq":
                                    ph.op("act", lambda e, bank=bank, ci=ci, cs=cs: e.activation(
                                        qpa[0:64, ci, cs], pb[bank][0:64, :], AF.Copy),
                                        reads=[("pb", bank), "qpa"], writes=[("qpa_d", ci, s)])
                                    ph.op("dve", lambda e, bank=bank, ci=ci, cs=cs: e.tensor_copy(
                                        qpa[64:128, 4 + ci, cs], pb[bank][64:128, :]),
                                        reads=[("pb", bank), "qpa"], writes=[("qpa_d", 4 + ci, s)])
                                elif kind == "bq":
                                    ph.op("act", lambda e, bank=bank, ci=ci, cs=cs: e.activation(
                                        qpb[0:64, 2 * ci, cs], pb[bank][0:64, :], AF.Copy),
                                        reads=[("pb", bank), "qpb"], writes=[("qpb_d", 2 * ci, s)])
                                    ph.op("dve", lambda e, bank=bank, ci=ci, cs=cs: e.tensor_copy(
                                        qpb[64:128, 2 * ci + 1, cs], pb[bank][64:128, :]),
                                        reads=[("pb", bank), "qpb"], writes=[("qpb_d", 2 * ci + 1, s)])
                                elif kind == "ak":
                                    ph.op("act", lambda e, bank=bank, gs=gs: e.activation(kaT[:, gs], pb[bank][:], AF.Copy),
                                          reads=[("pb", bank)], writes=[("kaT", s)])
                                else:
                                    ph.op("dve", lambda e, bank=bank, gs=gs, ci=ci: e.tensor_copy(kbT[:, ci, gs], pb[bank][:]),
                                          reads=[("pb", bank)], writes=[("kbT", ci, s)])
                            if kind == "bk":
                                nb0 = u * 4
                                ph.op("dve", lambda e, ci=ci, nb0=nb0: e.tensor_reduce(
                                    kmf[:, ci, nb0:nb0 + 4],
                                    kbT[:, ci, u * HALF:(u + 1) * HALF].rearrange("p (n t) -> p n t", n=4),
                                    AX.X, ALU.add),
                                    reads=[("kbT", ci, 0), ("kbT", ci, 1)], writes=[("kmf", ci)])
                                ph.op("dve", lambda e, ci=ci, nb0=nb0: e.tensor_scalar(
                                    kmT[:, ci, nb0:nb0 + 4], kmf[:, ci, nb0:nb0 + 4], 1.0 / 256.0, None, ALU.mult),
                                    reads=[("kmf", ci)], writes=[("kmT", ci)])
                        else:
                            for t4 in range(2):
                                bank = 5 + rots["v"] % 2
                                rots["v"] += 1

                                def mm(e, b=b, jj=jj, t4=t4, bank=bank):
                                    r = None
                                    for tt in range(4):
                                        tok = (t4 * 4 + tt) * 128
                                        for k in range(8):
                                            r = e.matmul(pb[bank][:, tt * 128:(tt + 1) * 128], hT[:, k, tok:tok + 128],
                                                         wbuf[b][:, jj, k, :], start=(k == 0), stop=(k == 7))
                                    return r
                                ph.op("pe", mm, reads=[("wbuf", b)] + hkeys(t4), writes=[("pb", bank)])
                                gt0 = u * 8 + t4 * 4
                                src = pb[bank][:].rearrange("p (t g d) -> p t g d", t=4, g=2)
                                if kind == "av":
                                    ph.op("act", lambda e, src=src, gt0=gt0: e.activation(
                                        Va[:, gt0:gt0 + 4, :, 0:64], src, AF.Copy),
                                        reads=[("pb", bank), "Va"], writes=[("Va_d", t4)])
                                else:
                                    ph.op("dve", lambda e, src=src, gt0=gt0, ci=ci: e.tensor_copy(
                                        Vb[:, gt0:gt0 + 4, 2 * ci:2 * ci + 2, 0:64], src),
                                        reads=[("pb", bank), "Vb"], writes=[("Vb_d", ci, t4)])
                stream(9, 3, issueA, computeA)
                emit_if(ph)

                plan.reset(lmark + 4 * K16)
                ph = Phase(nc, S_, f"B{l}{u}")
                gate = plan.alloc("gate", [128, 2, 8, 8], F32)
                top8 = plan.alloc("top8", [128, 2, 8, 8], F32)
                selb = plan.alloc("selb", [128, 2, 8, 8], BF16)
                selT = [plan.alloc(f"selT{i}", [128, 8, 256], BF16) for i in range(2)]
                pT = [plan.alloc(f"pT{i}", [128, 2, 256], BF16) for i in range(3)]
                pTa = [plan.alloc(f"pTa{i}", [128, 4, 128], BF16) for i in range(3)]
                otok = [plan.alloc(f"otok{i}", [128, 16, 64], BF16) for i in range(4)]
                den = [plan.alloc(f"den{i}", [128, 4, 1], F32) for i in range(4)]
                R = dict(s=0, o=0, p=0, pa=0, d=0)
                items = []
                if u == 1:
                    for i in range(2):
                        ph.op("pool", lambda e, i=i: e.memset(selT[i][:], 0.0), writes=[("selT", i, 0), ("selT", i, 1)])
                        ph.dma("pool", selT[i][8:10, :, :].rearrange("p h t -> p (h t)"), selc_d,
                               reads=[("selT", i, 0), ("selT", i, 1)], writes=[("selTc", i)])

                def normalize(br, g, jq, ob, ot, okey):
                    dn = den[R["d"] % 4]
                    dkey = ("den", R["d"] % 4)
                    R["d"] += 1
                    o3 = pb[ob][:, 0:260].rearrange("p (h d) -> p h d", h=4)
                    if br == "a":
                        ph.op("dve", lambda e: e.tensor_tensor(
                            dn[:], o3[:, :, 64:65], esink[:, l * 8 + 4 * g: l * 8 + 4 * g + 4].unsqueeze(2), ALU.add),
                            reads=[("pb", ob)], writes=[dkey])
                        ph.op("dve", lambda e: e.reciprocal(dn[:], dn[:]), reads=[dkey], writes=[dkey])
                    else:
                        ph.op("dve", lambda e: e.reciprocal(dn[:], o3[:, :, 64:65]), reads=[("pb", ob)], writes=[dkey])
                    h0 = (0 if br == "a" else 8) + 4 * g
                    ph.op("dve", lambda e: e.tensor_tensor(
                        ot[:, h0:h0 + 4, :], o3[:, :, 0:64], dn[:].broadcast_to([128, 4, 64]), ALU.mult),
                        reads=[("pb", ob), dkey], writes=[okey + (br, g)])

                def gating1(qn):
                    ownn = 4 * u + qn
                    tqn = qn * 256
                    ph.op("pool", lambda e: e.memset(gate[:], NEG), writes=["gate"])

                    def mmg(e):
                        r = None
                        for tt in range(2):
                            for h in range(8):
                                r = e.matmul(pb[2][:, (tt * 8 + h) * 8:(tt * 8 + h) * 8 + 8],
                                             qpb[:, h, tqn + tt * 128: tqn + tt * 128 + 128], kmT[:, h // 2, :],
                                             start=True, stop=True)
                        return r
                    ph.op("pe", mmg, reads=[], writes=["pb2"])
                    gsrc = pb[2][:, 0:128].rearrange("p (t h n) -> p t h n", t=2, h=8)
                    ph.op("dve", lambda e: e.tensor_copy(gate[:, :, :, 0:ownn], gsrc[:, :, :, 0:ownn]),
                          reads=["pb2", "gate"], writes=["gate"])
                    for tt in range(2):
                        for h in range(8):
                            ph.op("dve", lambda e, tt=tt, h=h: e.max(top8[:, tt, h, :], gate[:, tt, h, :]),
                                  reads=["gate"], writes=[("top8", tt, h)])
                            ph.op("dve", lambda e, tt=tt, h=h: e.tensor_scalar(
                                selb[:, tt, h, :], gate[:, tt, h, :], top8[:, tt, h, 2:3], 1.0, ALU.is_ge, ALU.subtract),
                                reads=["gate", ("top8", tt, h)], writes=[("selb", tt, h)])

                def gating3(qn):
                    st_ = selT[qn % 2]
                    for hg in range(2):
                        def tr(e, hg=hg):
                            r = None
                            dst = pb[2][:].bitcast(BF16).rearrange("p (h t) -> p h t", h=4)
                            for hh in range(4):
                                for tt in range(2):
                                    r = e.transpose(dst[0:8, hh, tt * 128:(tt + 1) * 128], selb[:, tt, hg * 4 + hh, :], ident)
                            return r
                        ph.op("pe", tr, reads=[("selb", tt, h) for tt in range(2) for h in range(hg * 4, hg * 4 + 4)],
                              writes=["pb2"])
                        srcT = pb[2][:].bitcast(BF16).rearrange("p (h t) -> p h t", h=4)
                        ph.op("dve", lambda e, hg=hg, srcT=srcT: e.tensor_copy(
                            st_[0:8, hg * 4:hg * 4 + 4, :], srcT[0:8, :, :]),
                            reads=["pb2"], writes=[("selT", qn % 2, hg)])

                def otranspose(qn):
                    for jq in range(2):
                        ot = otok[(qn % 2) * 2 + jq]
                        okey = ("otok", (qn % 2) * 2 + jq)

                        def trn(e, ot=ot):
                            r = None
                            dst = pb[2][:].bitcast(BF16).rearrange("p (c t) -> p c t", c=8)
                            for c in range(8):
                                r = e.transpose(dst[:, c, :], ot[:, 2 * c:2 * c + 2, :].rearrange("p a b -> p (a b)"), ident)
                            return r
                        ph.op("pe", trn, reads=[okey + (br, g) for br in ("a", "b") for g in range(2)], writes=["pb2"])
                        srcT = pb[2][:].bitcast(BF16).rearrange("p (c t) -> p c t", c=8)
                        tcol = qn * 256 + jq * 128
                        ph.op("dve", lambda e, srcT=srcT, tcol=tcol: e.tensor_copy(oT[:, :, tcol:tcol + 128], srcT),
                              reads=["pb2"], writes=[("oT", qn, jq)])

                for qb in range(4):
                    own = 4 * u + qb
                    tq0 = qb * 256
                    gating = own >= 4
                    selq = selT[qb % 2]
                    first_item_of_qb = len(items)
                    pre_ops = []
                    if gating and qb == 0:
                        pre_ops.append(lambda qb=qb: (gating1(qb), gating3(qb)))
                    nxt_gating = (qb + 1 < 4) and (4 * u + qb + 1 >= 4)
                    if nxt_gating:
                        pre_ops.append(lambda qb=qb: gating1(qb + 1))
                    for g in range(2):
                        obs = []
                        for jq in range(2):
                            obs.append(4 + R["o"] % 4)
                            R["o"] += 1
                        for jq in range(2):
                            ob = obs[jq]
                            i_glob = 2 * own + jq
                            ktiles = ([i_glob - 1] if i_glob > 0 else []) + [i_glob]
                            tqc = slice(tq0 + jq * 128, tq0 + jq * 128 + 128)
                            for ki, kt in enumerate(ktiles):
                                typ = 0 if kt == i_glob else 1
                                sbank = (0, 1, 3)[R["s"] % 3]
                                R["s"] += 1
                                pa = pTa[R["pa"] % 3]
                                pakey = ("pTa", R["pa"] % 3)
                                R["pa"] += 1

                                def S(g=g, kt=kt, typ=typ, sbank=sbank, tqc=tqc):
                                    def mms(e):
                                        r = None
                                        for hh in range(4):
                                            h = 4 * g + hh
                                            dst = pb[sbank][:, hh * 128:(hh + 1) * 128]
                                            e.matmul(dst, kaT[:, kt * 128:(kt + 1) * 128], qpa[:, h, tqc], start=True, stop=False)
                                            e.matmul(dst, ident, swam[:, h, typ, 0, :], start=False, stop=False)
                                            r = e.matmul(dst, ident, swam[:, h, typ, 1, :], start=False, stop=True)
                                        return r
                                    ph.op("pe", mms, reads=[], writes=[("sb", sbank)])

                                def E(sbank=sbank, pa=pa, pakey=pakey):
                                    ph.op("act", lambda e: e.activation(
                                        pa[:].rearrange("p a b -> p (a b)"), pb[sbank][:], AF.Exp, scale=0.125),
                                        reads=[("sb", sbank)], writes=[pakey])

                                def PV(g=g, kt=kt, pa=pa, pakey=pakey, ob=ob, first=(ki == 0), last=(ki == len(ktiles) - 1)):
                                    def mmpv(e):
                                        r = None
                                        for hh in range(4):
                                            r = e.matmul(pb[ob][:, hh * 65:hh * 65 + 65], pa[:, hh, :], Va[:, kt, g, :],
                                                         start=(first and hh == 0), stop=last, skip_group_check=True)
                                        return r
                                    ph.op("pe", mmpv, reads=[pakey], writes=[("pb", ob)])
                                items.append(dict(S=S, E=E, PV=PV, pre=[], post=[]))
                        post = items[-1]["post"]
                        for jq in range(2):
                            post.append(lambda g=g, jq=jq, ob=obs[jq], qb=qb: normalize(
                                "a", g, jq, ob, otok[(qb % 2) * 2 + jq], ("otok", (qb % 2) * 2 + jq)))
                        if g == 0:
                            if nxt_gating:
                                post.append(lambda qb=qb: gating3(qb + 1))
                            if qb > 0:
                                post.append(lambda qb=qb: otranspose(qb - 1))
                    for hg in range(2):
                        obs = []
                        for jq in range(2):
                            obs.append(4 + R["o"] % 4)
                            R["o"] += 1
                        for hh in range(4):
                            h = hg * 4 + hh
                            if gating:
                                for n in range(own):
                                    dist = own - n
                                    si = (0, 1, 3)[R["s"] % 3]
                                    R["s"] += 1
                                    pt = pT[R["p"] % 3]
                                    ptkey = ("pT", R["p"] % 3)
                                    R["p"] += 1

                                    def S2(h=h, n=n, si=si, tq0=tq0, selq=selq, qb=qb):
                                        def mmq(e):
                                            r = None
                                            for j in range(2):
                                                dst = pb[si][:, j * 256:(j + 1) * 256]
                                                kt = 2 * n + j
                                                e.matmul(dst, kbT[:, h // 2, kt * 128:(kt + 1) * 128], qpb[:, h, tq0:tq0 + 256],
                                                         start=True, stop=False)
                                                r = e.matmul(dst, indt[:, n, j, :], selq[:, h, :], start=False, stop=True)
                                            return r
                                        ph.op("pe", mmq, reads=[("selT", qb % 2, h // 4)], writes=[("sb", si)])

                                    def E2(si=si, pt=pt, ptkey=ptkey, bcol=h * 16 + dist * 2):
                                        ph.op("act", lambda e: e.activation(
                                            pt[:].rearrange("p j t -> p (j t)"), pb[si][:], AF.Exp,
                                            bias=alb[:, bcol:bcol + 1], scale=0.125),
                                            reads=[("sb", si)], writes=[ptkey])

                                    def PV2(h=h, hh=hh, n=n, pt=pt, ptkey=ptkey, obs=obs):
                                        def mmpv(e):
                                            r = None
                                            for j in range(2):
                                                for jq in range(2):
                                                    r = e.matmul(pb[obs[jq]][:, hh * 65:hh * 65 + 65],
                                                                 pt[:, j, jq * 128:(jq + 1) * 128], Vb[:, 2 * n + j, h, :],
                                                                 start=(n == 0 and j == 0), stop=False)
                                            return r
                                        ph.op("pe", mmpv, reads=[ptkey], writes=[("pb", obs[0]), ("pb", obs[1])])
                                    items.append(dict(S=S2, E=E2, PV=PV2, pre=[], post=[]))
                                kts = [(own, 0), (own, 1)]
                            else:
                                kts = [(n, j) for n in range(own) for j in range(2)] + [(own, 0), (own, 1)]
                            for (n, j) in kts:
                                dist = own - n
                                kt = n * 2 + j
                                si = (0, 1, 3)[R["s"] % 3]
                                R["s"] += 1
                                ps_s = pb[si][:, 0:256]
                                pt = pT[R["p"] % 3]
                                ptkey = ("pT", R["p"] % 3)
                                R["p"] += 1
                                c0 = 128 if (n == own and j == 1) else 0

                                def S(h=h, n=n, j=j, kt=kt, ps_s=ps_s, own=own, tq0=tq0, gating=gating, si=si, selq=selq, qb=qb):
                                    def mmq(e):
                                        lhs = kbT[:, h // 2, kt * 128:(kt + 1) * 128]
                                        if n < own:
                                            r = e.matmul(ps_s, lhs, qpb[:, h, tq0:tq0 + 256], start=True, stop=not gating)
                                            if gating:
                                                r = e.matmul(ps_s, indt[:, n, 0, :], selq[:, h, :], start=False, stop=True)
                                        else:
                                            d0 = j * 128
                                            e.matmul(ps_s[:, d0:d0 + 128], lhs, qpb[:, h, tq0 + d0:tq0 + d0 + 128],
                                                     start=True, stop=False)
                                            r = e.matmul(ps_s[:, d0:d0 + 128], ident, cmask, start=False, stop=True)
                                            if j == 0:
                                                r = e.matmul(ps_s[:, 128:256], lhs, qpb[:, h, tq0 + 128:tq0 + 256],
                                                             start=True, stop=True)
                                        return r
                                    rd = [("selT", qb % 2, h // 4)] if (gating and n < own) else []
                                    ph.op("pe", mmq, reads=rd, writes=[("sb", si)])

                                def E(ps_s=ps_s, pt=pt, ptkey=ptkey, c0=c0, si=si, bcol=h * 16 + dist * 2 + j):
                                    ph.op("act", lambda e: e.activation(
                                        pt[:, 0, c0:256], ps_s[:, c0:256], AF.Exp, bias=alb[:, bcol:bcol + 1], scale=0.125),
                                        reads=[("sb", si)], writes=[ptkey])

                                def PV(h=h, hh=hh, kt=kt, pt=pt, ptkey=ptkey, obs=obs, n=n, j=j, own=own):
                                    def mmpv(e):
                                        r = None
                                        first = (kt == 0)
                                        for jq in range(2):
                                            if n == own and j == 1 and jq == 0:
                                                continue
                                            last = (n == own and j == jq)
                                            r = e.matmul(pb[obs[jq]][:, hh * 65:hh * 65 + 65], pt[:, 0, jq * 128:(jq + 1) * 128],
                                                         Vb[:, kt, h, :], start=first, stop=last)
                                        return r
                                    ph.op("pe", mmpv, reads=[ptkey], writes=[("pb", obs[0]), ("pb", obs[1])])
                                items.append(dict(S=S, E=E, PV=PV, pre=[], post=[]))
                        post = items[-1]["post"]
                        for jq in range(2):
                            post.append(lambda hg=hg, jq=jq, ob=obs[jq], qb=qb: normalize(
                                "b", hg, jq, ob, otok[(qb % 2) * 2 + jq], ("otok", (qb % 2) * 2 + jq)))
                    items[first_item_of_qb]["pre"] = pre_ops
                items[-1]["post"].append(lambda: otranspose(3))
                LA = 2
                for idx in range(len(items) + LA):
                    if idx < len(items):
                        for f in items[idx]["pre"]:
                            f()
                        items[idx]["S"]()
                        items[idx]["E"]()
                    k = idx - LA
                    if k >= 0:
                        items[k]["PV"]()
                        for f in items[k]["post"]:
                            f()
                emit_if(ph)

                plan.reset(lmark + 2 * K16)
                ph = Phase(nc, S_, f"C1{l}{u}")
                merged = plan.alloc("merged", [128, 8, HALF], BF16)
                wA = [plan.alloc(f"wA{i}", [128, 2, 8, 128], BF16) for i in range(2)]
                wB = [plan.alloc(f"wB{i}", [128, 2, 2, 4, 128], BF16) for i in range(2)]
                tg = [plan.alloc(f"tg{i}", [128, 512], F32) for i in range(4)]
                mt = [plan.alloc(f"mt{i}", [128, 512], F32) for i in range(4)]
                R = dict(r=0)

                def issueC1(j):
                    ab = j % 2
                    ph.dma("pool", wA[ab][:].rearrange("p a k n -> p (a k n)"), wtl_d[l * N_TAIL + j], writes=[("wA", ab)])
                    if j % 2 == 0:
                        bb = (j // 2) % 2
                        ph.dma("pool", wB[bb][:].rearrange("p a b k n -> p (a b k n)"), wtl_d[l * N_TAIL + 8 + j // 2],
                               writes=[("wB", bb)])

                def computeC1(j):
                    ab, bb, jp = j % 2, (j // 2) % 2, j % 2
                    for s in range(2):
                        r4 = R["r"] % 2
                        R["r"] += 1
                        bga, bgb, boa, bob = 4 * r4, 4 * r4 + 1, 4 * r4 + 2, 4 * r4 + 3
                        cs = slice(s * 512, (s + 1) * 512)

                        def mm(e, ab=ab, bb=bb, jp=jp, cs=cs, bga=bga, bgb=bgb, boa=boa, bob=bob):
                            r = None
                            for k in range(8):
                                e.matmul(pb[bga][:], wA[ab][:, 0, k, :], hT[:, k, cs], start=(k == 0), stop=(k == 7))
                            for k in range(8):
                                e.matmul(pb[bgb][:], wA[ab][:, 1, k, :], hT[:, k, cs], start=(k == 0), stop=(k == 7))
                            for k in range(4):
                                e.matmul(pb[boa][:], wB[bb][:, jp, 0, k, :], oT[:, k, cs], start=(k == 0), stop=(k == 3))
                            for k in range(4):
                                r = e.matmul(pb[bob][:], wB[bb][:, jp, 1, k, :], oT[:, 4 + k, cs], start=(k == 0), stop=(k == 3))
                            return r
                        ph.op("pe", mm, reads=[("wA", ab), ("wB", bb)], writes=[("pbg", r4)])
                        ta, tb2 = tg[2 * r4], tg[2 * r4 + 1]
                        m1, m2 = mt[2 * r4], mt[2 * r4 + 1]
                        ph.op("act", lambda e, ta=ta, bga=bga: e.activation(ta[:], pb[bga][:], AF.Tanh, scale=0.5),
                              reads=[("pbg", r4)], writes=[("tg", 2 * r4)])
                        ph.op("act", lambda e, tb2=tb2, bgb=bgb: e.activation(tb2[:], pb[bgb][:], AF.Tanh, scale=0.5),
                              reads=[("pbg", r4)], writes=[("tg", 2 * r4 + 1)])
                        ph.op("dve", lambda e, ta=ta, m1=m1, boa=boa: e.scalar_tensor_tensor(
                            m1[:], ta[:], 1.0, pb[boa][:], ALU.add, ALU.mult),
                            reads=[("tg", 2 * r4), ("pbg", r4)], writes=[("mt", 2 * r4)])
                        ph.op("dve", lambda e, tb2=tb2, m2=m2, bob=bob: e.scalar_tensor_tensor(
                            m2[:], tb2[:], 1.0, pb[bob][:], ALU.add, ALU.mult),
                            reads=[("tg", 2 * r4 + 1), ("pbg", r4)], writes=[("mt", 2 * r4 + 1)])
                        ph.op("dve", lambda e, m1=m1, m2=m2, j=j, cs=cs: e.tensor_tensor(merged[:, j, cs], m1[:], m2[:], ALU.add),
                              reads=[("mt", 2 * r4), ("mt", 2 * r4 + 1)], writes=[("merged", j, s)])
                stream(8, 2, issueC1, computeC1)
                emit_if(ph)

                plan.reset(lmark)
                ph = Phase(nc, S_, f"C2{l}{u}")
                wout = plan.alloc("wout", [128, 4, 2, 8, 128], BF16)
                y = plan.alloc("y", [128, 8, 512], F32)
                plan.reset(lmark + 3 * K16)
                sq2 = plan.alloc("sq8", [128, 8, 512], BF16)
                sdt = plan.alloc("sdt", [128, 512], F32)
                rstd = plan.alloc("rstd", [128, 512], F32)
                tmp = [plan.alloc(f"tmp{i}", [128, 512], F32) for i in range(4)]
                for pp in range(4):
                    ph.dma("pool", wout[:, pp].rearrange("p a k n -> p (a k n)"), wtl_d[l * N_TAIL + 12 + pp],
                           writes=[("wout", pp)])
                nxt = l + 1 < n_layers
                if nxt:
                    abuf2 = [plan.alloc(f"abuf2{i}", [128, 2, 8, 128], BF16) for i in range(3)]

                    def ada_issue(i):
                        ph.dma("pool", abuf2[i % 3][:].rearrange("p a k n -> p (a k n)"),
                               ada_d[(l + 1) * 24 + u * 12 + i], writes=[("abuf2", i % 3)])

                    def ada_mm(i):
                        def mm(e, i=i):
                            r = None
                            for jj in range(2):
                                col = i * 2 + jj
                                for k in range(8):
                                    r = e.matmul(pb[5][:, col:col + 1], abuf2[i % 3][:, jj, k, :], cond[:, k:k + 1],
                                                 start=(k == 0), stop=(k == 7))
                            return r
                        ph.op("pe", mm, reads=[("abuf2", i % 3)], writes=["pb5"])
                    for i in range(3):
                        ada_issue(i)
                for s in range(2):
                    cs = slice(s * 512, (s + 1) * 512)

                    def mmf(e, j, bank, cs=cs):
                        r = None
                        for k in range(8):
                            r = e.matmul(pb[bank][:], wout[:, j // 2, j % 2, k, :], merged[:, k, cs],
                                         start=(k == 0), stop=(k == 7))
                        return r

                    def hook(j, s=s):
                        i = s * 8 + j
                        if nxt and i < 12:
                            ada_mm(i)
                            if i + 3 < 12:
                                ada_issue(i + 3)
                    ydown(ph, mmf, (lambda j: [("wout", j // 2)]), y, sq2, l, 2, 2 * u + s, sdt, rstd, tmp, 4.0 * D, 0,
                          hook=hook)
                if nxt:
                    c0 = (l + 1) * 48 + u * 24
                    ph.op("dve", lambda e, c0=c0: e.tensor_tensor(
                        modv[:, c0:c0 + 24], pb[5][:, 0:24], smalls[:, SM_ADAB + c0:SM_ADAB + c0 + 24], ALU.add),
                        reads=["pb5"], writes=["modv"])
                    if u == 1:
                        derive_vecs(ph, l + 1)
                emit_if(ph)
            for u in range(2):
                plan.reset(pmark)
                actT = plan.alloc("actT", [128, NF, HALF], BF16)
                fmark = plan.mark()
                ph = Phase(nc, S_, f"F1{l}{u}")
                h2T = plan.alloc("h2T", [128, 8, HALF], BF16)
                sqs = [plan.alloc(f"sqs{i}", [128, 512], BF16) for i in range(2)]
                sdt = [plan.alloc(f"sdt{i}", [128, 512], F32) for i in range(1)]
                rstd = [plan.alloc(f"rstd{i}", [128, 512], F32) for i in range(2)]
                tmp = [plan.alloc(f"tmp{i}", [128, 512], F32) for i in range(2)]
                wg = [plan.alloc(f"wg{i}", [128, 2, 8, 128], BF16) for i in range(3)]
                sg = [plan.alloc(f"sg{i}", [128, 512], F32) for i in range(3)]
                R = dict(r=0)

                def issueF1(f):
                    b = f % 3
                    ph.dma("pool", wg[b][:].rearrange("p a k n -> p (a k n)"), wgu_d[l * N_GU + f], writes=[("wg", b)])

                def computeF1(f):
                    b = f % 3
                    if f == 0:
                        prenorm_both(ph, l, 3, 4, h2T, u, sqs, sdt, rstd, tmp)
                    for s in range(2):
                        r3 = R["r"] % 3
                        R["r"] += 1
                        bg, bu = 1 + 2 * r3, 2 + 2 * r3
                        cs = slice(s * 512, (s + 1) * 512)

                        def mm(e, b=b, cs=cs, bg=bg, bu=bu):
                            r = None
                            for k in range(8):
                                e.matmul(pb[bg][:], wg[b][:, 0, k, :], h2T[:, k, cs], start=(k == 0), stop=(k == 7))
                            for k in range(8):
                                r = e.matmul(pb[bu][:], wg[b][:, 1, k, :], h2T[:, k, cs], start=(k == 0), stop=(k == 7))
                            return r
                        ph.op("pe", mm, reads=[("wg", b)] + hkeys(s), writes=[("pbp", r3)])
                        sgt = sg[r3]
                        ph.op("act", lambda e, sgt=sgt, bg=bg: e.activation(sgt[:], pb[bg][:], AF.Silu),
                              reads=[("pbp", r3)], writes=[("sg", r3)])
                        ph.op("dve", lambda e, sgt=sgt, bu=bu, f=f, cs=cs: e.tensor_tensor(actT[:, f, cs], sgt[:], pb[bu][:], ALU.mult),
                              reads=[("sg", r3), ("pbp", r3)], writes=[("actT", f, s)])
                stream(NF, 3, issueF1, computeF1)
                emit_if(ph)
                plan.reset(fmark)
                ph = Phase(nc, S_, f"F2{l}{u}")
                wdn = plan.alloc("wdn", [128, 8, NF, 128], BF16)
                y = plan.alloc("y", [128, 8, 512], F32)
                sq2 = plan.alloc("sq8", [128, 8, 512], BF16)
                sdt = plan.alloc("sdt", [128, 512], F32)
                rstd = plan.alloc("rstd", [128, 512], F32)
                tmp = [plan.alloc(f"tmp{i}", [128, 512], F32) for i in range(4)]
                for pp in range(N_WD):
                    ph.dma("pool", wdn[:, pp // 2, (pp % 2) * 11:(pp % 2) * 11 + 11, :].rearrange("p f n -> p (f n)"),
                           wd_d[l * N_WD + pp], writes=[("wdn", pp)])
                for s in range(2):
                    cs = slice(s * 512, (s + 1) * 512)

                    def mmf(e, j, bank, cs=cs):
                        r = None
                        for f in range(NF):
                            r = e.matmul(pb[bank][:], wdn[:, j, f, :], actT[:, f, cs],
                                         start=(f == 0), stop=(f == NF - 1))
                        return r
                    ydown(ph, mmf, (lambda j: [("wdn", 2 * j), ("wdn", 2 * j + 1)]), y, sq2, l, 5, 2 * u + s, sdt, rstd, tmp, 1.0 * D, 0,
                          store=(l == n_layers - 1 and max_ph is None))
                emit_if(ph)
        if max_ph is not None:
            ph = Phase(nc, S_, "out")
            for c in range(8):
                ph.dma("sp", y_d[:, c * SEQ:(c + 1) * SEQ], xT[:, c, :], reads=[], writes=[("y_d", c)])
            ph.emit()
    return nc


def _kmaj(W, cols=None):
    if cols is not None:
        W = W[:, cols]
    K, N = W.shape
    return np.ascontiguousarray(W.reshape(K // 128, 128, N).transpose(1, 0, 2))


def _consts():
    i = np.arange(1, 17, dtype=np.float32)
    sl = (2.0 ** (-8.0 * i / 16)).astype(np.float32)
    sa, sb = sl[:8], sl[8:]
    p = np.arange(128)[:, None]
    f = np.arange(128)[None, :]
    ident = (p == f).astype(np.float32)
    cmask = np.where(p <= f, 0.0, -BIG).astype(np.float32)
    ones = np.ones((128, 128), np.float32)
    cst = np.concatenate([ident, cmask, ones], axis=1)
    swam = np.zeros((128, 8, 2, 2, 128), np.float32)
    for h in range(8):
        for typ in range(2):
            if typ == 0:
                valid = p <= f
                dist = (f - p).astype(np.float64)
            else:
                valid = p > f
                dist = (128 + f - p).astype(np.float64)
            M = np.where(valid, -8.0 * float(sa[h]) * dist, -BIG)
            hi = M.astype(np.float32).astype(ml_dtypes.bfloat16).astype(np.float32)
            lo = (M - hi).astype(np.float32).astype(ml_dtypes.bfloat16).astype(np.float32)
            swam[:, h, typ, 0, :] = hi
            swam[:, h, typ, 1, :] = lo
    alb = np.zeros((128, 128), np.float32)
    for h in range(8):
        for dist in range(8):
            for j in range(2):
                alb[:, h * 16 + dist * 2 + j] = sb[h] * (np.arange(128) + 128 * j - 256 * dist)
    ind = np.zeros((128, 8, 2, 128), np.float32)
    for n in range(8):
        ind[n, n, :, :] = BIG
    ind[8, :, 1, :] = 1.0
    ind[9, :, 1, :] = 1.0
    selc = np.zeros((2, 8, 256), np.float32)
    for h in range(8):
        v = 1024.0 * float(sb[h])
        hi = np.float32(v).astype(ml_dtypes.bfloat16).astype(np.float32)
        lo = np.float32(v - hi).astype(ml_dtypes.bfloat16).astype(np.float32)
        selc[0, h, :] = hi
        selc[1, h, :] = lo
    return cst, swam.reshape(128, 4096), alb, ind.reshape(128, 2048), selc.reshape(2, 2048)


def _prep_weights(ada_w, ada_b, norm_pre_mix, norm_post_mix, w_in, attn_sinks, w_o_a, w_o_b, w_out,
                  norm_pre_ffn, norm_post_ffn, w_gate_up, w_down):
    f32 = lambda a: np.asarray(a, dtype=np.float32)
    ada_w, w_in, w_o_a, w_o_b, w_out, w_gate_up, w_down = map(f32, (ada_w, w_in, w_o_a, w_o_b, w_out, w_gate_up, w_down))
    sm = np.zeros((128, SM_W), np.float32)
    for l in range(L):
        sm[:, SM_ADAB + l * 48: SM_ADAB + (l + 1) * 48] = f32(ada_b[l]).reshape(48, 128).T
        for gi, gn in enumerate((norm_pre_mix, norm_post_mix, norm_pre_ffn, norm_post_ffn)):
            sm[:, SM_GAIN + l * 32 + gi * 8: SM_GAIN + l * 32 + gi * 8 + 8] = f32(gn[l]).reshape(8, 128).T
        sm[:, SM_SINK + l * 8: SM_SINK + l * 8 + 8] = f32(attn_sinks[l])[None, :]
    ada = np.zeros((L * 24, 128, 2, 8, 128), np.float32)
    for l in range(L):
        A = _kmaj(ada_w[l]).reshape(128, 8, 48, 128)
        A = A.transpose(2, 0, 1, 3)
        ada[l * 24:(l + 1) * 24] = A.reshape(24, 2, 128, 8, 128).transpose(0, 2, 1, 3, 4)
    ada = ada.reshape(L * 24, 128, 2048)
    off = np.cumsum([0, 512, 128, 128, 512, 512, 512, 1024, 1024])
    o_aq, o_ak, o_av, o_bq, o_bk, o_bv, o_ga, o_gb = off[:8]
    chunks = []
    for c in range(4):
        chunks.append(np.concatenate([o_aq + c * 64 + np.arange(64), o_aq + (4 + c) * 64 + np.arange(64)]))
    chunks.append(o_ak + np.arange(128))
    chunks.append(o_av + np.arange(128))
    for c in range(4):
        chunks.append(o_bq + c * 128 + np.arange(128))
    for c in range(4):
        chunks.append(o_bk + c * 128 + np.arange(128))
    for c in range(4):
        chunks.append(o_bv + c * 128 + np.arange(128))
    win = np.zeros((L * N_WIN, 128, 2, 8, 128), np.float32)
    wtl = np.zeros((L * N_TAIL, 128, 2048), np.float32)
    wgu = np.zeros((L * N_GU, 128, 2, 8, 128), np.float32)
    wdd = np.zeros((L * N_WD, 128, 11, 128), np.float32)
    for l in range(L):
        for ci, cols in enumerate(chunks):
            win[l * N_WIN + ci // 2, :, ci % 2] = _kmaj(w_in[l], cols)
        for j in range(8):
            t = np.zeros((128, 2, 8, 128), np.float32)
            t[:, 0] = _kmaj(w_in[l], o_ga + j * 128 + np.arange(128))
            t[:, 1] = _kmaj(w_in[l], o_gb + j * 128 + np.arange(128))
            wtl[l * N_TAIL + j] = t.reshape(128, 2048)
        for jj in range(4):
            t = np.zeros((128, 2, 2, 4, 128), np.float32)
            for jp in range(2):
                j = 2 * jj + jp
                t[:, jp, 0] = _kmaj(w_o_a[l], j * 128 + np.arange(128))
                t[:, jp, 1] = _kmaj(w_o_b[l], j * 128 + np.arange(128))
            wtl[l * N_TAIL + 8 + jj] = t.reshape(128, 2048)
        for pp in range(4):
            t = np.zeros((128, 2, 8, 128), np.float32)
            for jp in range(2):
                t[:, jp] = _kmaj(w_out[l], (2 * pp + jp) * 128 + np.arange(128))
            wtl[l * N_TAIL + 12 + pp] = t.reshape(128, 2048)
        for f in range(NF):
            wgu[l * N_GU + f, :, 0] = _kmaj(w_gate_up[l], f * 128 + np.arange(128))
            wgu[l * N_GU + f, :, 1] = _kmaj(w_gate_up[l], DFF + f * 128 + np.arange(128))
        W4 = w_down[l].reshape(2, 11, 128, 8, 128)
        wdd[l * N_WD:(l + 1) * N_WD] = W4.transpose(3, 0, 2, 1, 4).reshape(16, 128, 11, 128)
    cst, swam, alb, ind, selc = _consts()
    return dict(smalls=sm, ada=ada, win=win.reshape(L * N_WIN, 128, 2048), wtail=wtl,
                wgu=wgu.reshape(L * N_GU, 128, 2048), wd=wdd.reshape(L * N_WD, 128, 1408),
                cst=cst, swam=swam, alibib=alb, ind=ind, selc=selc)


def make_in_maps(x, c, **w):
    shared = _prep_weights(**w)
    x = np.asarray(x, dtype=np.float32)
    c = np.asarray(c, dtype=np.float32)
    maps = []
    for b in range(x.shape[0]):
        xT = np.ascontiguousarray(x[b].T.reshape(8, 128, SEQ).transpose(1, 0, 2)).reshape(128, 8 * SEQ)
        cT = np.ascontiguousarray(c[b].reshape(8, 128).T)
        m = dict(shared)
        m["xT"] = xT
        m["cT"] = cT
        maps.append(m)
    return maps


def unpack_out(yT):
    return np.ascontiguousarray(yT.reshape(128, 8, SEQ).transpose(2, 1, 0).reshape(SEQ, D))


_NC_CACHE = {}


def kernel(x, c, ada_w, ada_b, norm_pre_mix, norm_post_mix, w_in, attn_sinks, w_o_a, w_o_b, w_out,
           norm_pre_ffn, norm_post_ffn, w_gate_up, w_down):
    maps = make_in_maps(x, c, ada_w=ada_w, ada_b=ada_b, norm_pre_mix=norm_pre_mix, norm_post_mix=norm_post_mix,
                        w_in=w_in, attn_sinks=attn_sinks, w_o_a=w_o_a, w_o_b=w_o_b, w_out=w_out,
                        norm_pre_ffn=norm_pre_ffn, norm_post_ffn=norm_post_ffn, w_gate_up=w_gate_up, w_down=w_down)
    if "nc" not in _NC_CACHE:
        _NC_CACHE["nc"] = build_program(L)
    nc = _NC_CACHE["nc"]
    res = run_bass_kernel_spmd(nc, maps, core_ids=list(range(8)))
    out = np.stack([unpack_out(np.asarray(r["yT"])) for r in res.results], axis=0)
    return out.astype(np.float32)
```
